# Optimizing a Trainium2 kernel written in Bass

```python
import jax, jax.numpy as jnp
from jax import lax
import numpy as np

D_MODEL = 1024
BATCH = 8
SEQ = 4096
DEPTH = 4

D_MIX = D_MODEL
W_GROUP = D_MIX // 4
HEAD_DIM = 64
ROT_DIM = HEAD_DIM // 4
ROPE_THETA = 500000.0
Q_BLOCK = 128
EPS = 1e-6
DSA_HEADS = W_GROUP // HEAD_DIM
IDX_HEADS = 8
IDX_DIM = 32
IDX_ROT = IDX_DIM // 4
DSA_TOPK = 256
NSA_HEADS = W_GROUP // HEAD_DIM
CMP_LEN = 32
CMP_STRIDE = 16
SLC_BLOCK = 64
SLC_TOPN = 16
WINDOW = 512
CONV_WIDTH = 3
POOL_GROUPS = 4
POOL_WINDOWS = (2, 4, 8, 16)
POOL_CH = W_GROUP // POOL_GROUPS

IN_SPLITS = (
    ("a_q", DSA_HEADS * HEAD_DIM), ("a_k", HEAD_DIM), ("a_v", HEAD_DIM),
    ("a_qi", IDX_HEADS * IDX_DIM), ("a_ki", IDX_DIM), ("a_wi", IDX_HEADS), ("a_gate", W_GROUP),
    ("b_q", NSA_HEADS * HEAD_DIM), ("b_kc", HEAD_DIM), ("b_vc", HEAD_DIM), ("b_ks", HEAD_DIM),
    ("b_vs", HEAD_DIM), ("b_kw", HEAD_DIM), ("b_vw", HEAD_DIM), ("b_g", 3 * NSA_HEADS), ("b_gate", W_GROUP),
    ("c_b", W_GROUP), ("c_c", W_GROUP), ("c_x", W_GROUP), ("c_gate", W_GROUP),
    ("d_u", W_GROUP), ("d_gate", W_GROUP),
)
D_IN = sum(w for _, w in IN_SPLITS)

kernel_name = "hybrid_parallel_dsa_nsa_conv_pool"


def split_cols(z):
    offs = np.cumsum([w for _, w in IN_SPLITS])[:-1].tolist()
    return dict(zip([n for n, _ in IN_SPLITS], jnp.split(z, offs, axis=-1)))


def rmsnorm(x, g):
    xf = x.astype(jnp.float32)
    y = xf * lax.rsqrt(jnp.mean(xf * xf, axis=-1, keepdims=True) + EPS)
    return (y * g.astype(jnp.float32)).astype(x.dtype)


def rope(x, pos, rot):
    half = rot // 2
    inv = ROPE_THETA ** (-jnp.arange(half, dtype=jnp.float32) / half)
    ang = pos.astype(jnp.float32)[:, None] * inv[None, :]
    cos = jnp.cos(ang)[:, None, :].astype(x.dtype)
    sin = jnp.sin(ang)[:, None, :].astype(x.dtype)
    x1, x2, rest = x[..., :half], x[..., half:rot], x[..., rot:]
    return jnp.concatenate([x1 * cos - x2 * sin, x2 * cos + x1 * sin, rest], axis=-1)


def masked_softmax(s, valid):
    s = jnp.where(valid, s.astype(jnp.float32), -jnp.inf)
    m = jnp.max(s, axis=-1, keepdims=True)
    m = jnp.where(jnp.isfinite(m), m, 0.0)
    e = jnp.where(valid, jnp.exp(s - m), 0.0)
    return e / jnp.maximum(jnp.sum(e, axis=-1, keepdims=True), 1e-30)


take_rows = jax.vmap(lambda a, i: a[i])


def dsa_mixer(q, k, v, qi, ki, wi):
    B, T = q.shape[:2]
    topk = min(DSA_TOPK, T // 4)
    key_pos = jnp.arange(T)

    def block(i):
        q0 = i * Q_BLOCK
        t = q0 + jnp.arange(Q_BLOCK)
        qb = lax.dynamic_slice_in_dim(q, q0, Q_BLOCK, axis=1)
        qib = lax.dynamic_slice_in_dim(qi, q0, Q_BLOCK, axis=1)
        wib = lax.dynamic_slice_in_dim(wi, q0, Q_BLOCK, axis=1) * (IDX_HEADS ** -0.5)
        logits = jnp.einsum('bqhd,bsd->bqhs', qib, ki) * (IDX_DIM ** -0.5)
        score = jnp.einsum('bqhs,bqh->bqs', jax.nn.relu(logits), wib).astype(jnp.float32)
        causal = key_pos[None, :] <= t[:, None]
        score = jnp.where(causal[None], score, -jnp.inf)
        _, idx = lax.top_k(score, topk)
        k_sel = take_rows(k, idx)
        v_sel = take_rows(v, idx)
        s = jnp.einsum('bqhd,bqkd->bhqk', qb, k_sel) * (HEAD_DIM ** -0.5)
        valid = (idx <= t[None, :, None])[:, None]
        p = masked_softmax(s, valid).astype(v.dtype)
        return jnp.einsum('bhqk,bqkd->bqhd', p, v_sel)

    out = lax.map(block, jnp.arange(T // Q_BLOCK))
    return jnp.moveaxis(out, 0, 1).reshape(B, T, -1)


def nsa_mixer(q, kc_tok, vc_tok, ks, vs, kw, vw, gates, pe_cmp, w_cmp_k, w_cmp_v):
    B, T = q.shape[:2]
    dtype = q.dtype
    n_cmp = (T - CMP_LEN) // CMP_STRIDE + 1
    n_slc = T // SLC_BLOCK
    topn = min(SLC_TOPN, n_slc)
    scale = HEAD_DIM ** -0.5
    cmp_start = jnp.arange(n_cmp) * CMP_STRIDE
    cmp_end = cmp_start + CMP_LEN - 1
    cmp_idx = cmp_start[:, None] + jnp.arange(CMP_LEN)[None, :]

    def compress(tok, w):
        blocks = tok[:, cmp_idx] + pe_cmp
        return blocks.reshape(B, n_cmp, CMP_LEN * HEAD_DIM) @ w

    k_cmp = rope(compress(kc_tok, w_cmp_k)[:, :, None], cmp_end, ROT_DIM)[:, :, 0]
    v_cmp = compress(vc_tok, w_cmp_v)
    slc_start = jnp.arange(n_slc) * SLC_BLOCK
    overlap = ((cmp_start[:, None] < slc_start[None, :] + SLC_BLOCK)
               & (cmp_end[:, None] >= slc_start[None, :])).astype(jnp.float32)
    ks_blocks = ks.reshape(B, n_slc, SLC_BLOCK, HEAD_DIM)
    vs_blocks = vs.reshape(B, n_slc, SLC_BLOCK, HEAD_DIM)
    kw_pad = jnp.pad(kw, ((0, 0), (WINDOW, 0), (0, 0)))
    vw_pad = jnp.pad(vw, ((0, 0), (WINDOW, 0), (0, 0)))
    blk_ids = jnp.arange(n_slc)
    in_blk = jnp.arange(SLC_BLOCK)

    def block(i):
        q0 = i * Q_BLOCK
        t = q0 + jnp.arange(Q_BLOCK)
        qb = lax.dynamic_slice_in_dim(q, q0, Q_BLOCK, axis=1)
        gb = lax.dynamic_slice_in_dim(gates, q0, Q_BLOCK, axis=1)
        s_c = jnp.einsum('bqhd,bnd->bhqn', qb, k_cmp) * scale
        valid_c = (cmp_end[None, :] <= t[:, None])[None, None]
        p_c = masked_softmax(s_c, valid_c)
        o_c = jnp.einsum('bhqn,bnd->bqhd', p_c.astype(dtype), v_cmp)
        imp = jnp.einsum('bhqn,nj->bqj', p_c, overlap)
        blk_t = t // SLC_BLOCK
        forced = (blk_ids[None, :] == 0) | (blk_ids[None, :] == blk_t[:, None])
        admissible = blk_ids[None, :] <= blk_t[:, None]
        imp = jnp.where(forced, jnp.inf, jnp.where(admissible, imp, -jnp.inf))
        _, idx = lax.top_k(imp, topn)
        k_sel = take_rows(ks_blocks, idx).reshape(B, Q_BLOCK, topn * SLC_BLOCK, HEAD_DIM)
        v_sel = take_rows(vs_blocks, idx).reshape(B, Q_BLOCK, topn * SLC_BLOCK, HEAD_DIM)
        pos_sel = (idx[..., None] * SLC_BLOCK + in_blk).reshape(B, Q_BLOCK, topn * SLC_BLOCK)
        s_s = jnp.einsum('bqhd,bqkd->bhqk', qb, k_sel) * scale
        p_s = masked_softmax(s_s, (pos_sel <= t[None, :, None])[:, None]).astype(dtype)
        o_s = jnp.einsum('bhqk,bqkd->bqhd', p_s, v_sel)
        kwb = lax.dynamic_slice_in_dim(kw_pad, q0, Q_BLOCK + WINDOW, axis=1)
        vwb = lax.dynamic_slice_in_dim(vw_pad, q0, Q_BLOCK + WINDOW, axis=1)
        pos_w = q0 - WINDOW + jnp.arange(Q_BLOCK + WINDOW)
        valid_w = ((pos_w[None, :] >= 0) & (pos_w[None, :] <= t[:, None])
                   & (t[:, None] - pos_w[None, :] < WINDOW))
        s_w = jnp.einsum('bqhd,bkd->bhqk', qb, kwb) * scale
        p_w = masked_softmax(s_w, valid_w[None, None]).astype(dtype)
        o_w = jnp.einsum('bhqk,bkd->bqhd', p_w, vwb)
        g = jax.nn.sigmoid(gb.astype(jnp.float32)).astype(dtype)
        return g[..., 0:1] * o_c + g[..., 1:2] * o_s + g[..., 2:3] * o_w

    out = lax.map(block, jnp.arange(T // Q_BLOCK))
    return jnp.moveaxis(out, 0, 1).reshape(B, T, -1)


def shortconv_mixer(b_gate, c_gate, xc, conv_w):
    T = xc.shape[1]
    u = c_gate * xc
    up = jnp.pad(u, ((0, 0), (CONV_WIDTH - 1, 0), (0, 0)))
    y = up[:, 0:T] * conv_w[0]
    for j in range(1, CONV_WIDTH):
        y = y + up[:, j:j + T] * conv_w[j]
    return b_gate * y


def pool_mixer(u, pool_w, pool_scale):
    B, T, _ = u.shape
    ug = u.reshape(B, T, POOL_GROUPS, POOL_CH)
    cs = jnp.cumsum(ug.astype(jnp.float32), axis=1)
    cs = jnp.pad(cs, ((0, 0), (1, 0), (0, 0), (0, 0)))
    win = jnp.array(POOL_WINDOWS, dtype=jnp.int32)
    t = jnp.arange(T)
    start = jnp.maximum(t[:, None] + 1 - win[None, :], 0)
    lower = cs[:, start, jnp.arange(POOL_GROUPS)[None, :]]
    count = jnp.minimum(t[:, None] + 1, win[None, :]).astype(jnp.float32)
    mean = (cs[:, 1:] - lower) / count[None, :, :, None]
    pooled = (mean - ug.astype(jnp.float32)).astype(u.dtype)
    y = jnp.einsum('btgc,gcd->btgd', pooled, pool_w).reshape(B, T, W_GROUP)
    return y * pool_scale


def hybrid_layer(x, norm_w, w_in, w_out, conv_w, pe_cmp, w_cmp_k, w_cmp_v, pool_w, pool_scale):
    B, T, _ = x.shape
    pos = jnp.arange(T)
    h = rmsnorm(x, norm_w)
    p = split_cols(h @ w_in)
    heads = lambda a, n: a.reshape(B, T, n, -1)
    one_head_rope = lambda a, rot: rope(a[:, :, None], pos, rot)[:, :, 0]
    o_a = dsa_mixer(rope(heads(p['a_q'], DSA_HEADS), pos, ROT_DIM), one_head_rope(p['a_k'], ROT_DIM),
                    p['a_v'], rope(heads(p['a_qi'], IDX_HEADS), pos, IDX_ROT),
                    one_head_rope(p['a_ki'], IDX_ROT), p['a_wi'])
    o_b = nsa_mixer(rope(heads(p['b_q'], NSA_HEADS), pos, ROT_DIM), p['b_kc'], p['b_vc'],
                    one_head_rope(p['b_ks'], ROT_DIM), p['b_vs'],
                    one_head_rope(p['b_kw'], ROT_DIM), p['b_vw'],
                    heads(p['b_g'], NSA_HEADS), pe_cmp, w_cmp_k, w_cmp_v)
    o_c = shortconv_mixer(p['c_b'], p['c_c'], p['c_x'], conv_w)
    o_d = pool_mixer(p['d_u'], pool_w, pool_scale)
    mixed = jnp.concatenate([jax.nn.silu(p['a_gate']) * o_a, jax.nn.silu(p['b_gate']) * o_b,
                             jax.nn.silu(p['c_gate']) * o_c, jax.nn.silu(p['d_gate']) * o_d], axis=-1)
    return x + mixed @ w_out


def setup_inputs(seed: int = 0) -> dict:
    key = jax.random.key(seed)
    ks = jax.random.split(key, 11)
    f32 = jnp.float32
    x = jax.random.normal(ks[0], (BATCH, SEQ, D_MODEL), f32)
    norm_w = 1.0 + 0.1 * jax.random.normal(ks[1], (DEPTH, D_MODEL), f32)
    w_in = jax.random.normal(ks[2], (DEPTH, D_MODEL, D_IN), f32) * D_MODEL ** -0.5
    w_out = jax.random.normal(ks[3], (DEPTH, D_MIX, D_MODEL), f32) * D_MIX ** -0.5
    conv_w = jax.random.normal(ks[4], (DEPTH, CONV_WIDTH, W_GROUP), f32) * CONV_WIDTH ** -0.5
    pe_cmp = 0.02 * jax.random.normal(ks[5], (DEPTH, CMP_LEN, HEAD_DIM), f32)
    w_cmp_k = jax.random.normal(ks[6], (DEPTH, CMP_LEN * HEAD_DIM, HEAD_DIM), f32) * (CMP_LEN * HEAD_DIM) ** -0.5
    w_cmp_v = jax.random.normal(ks[7], (DEPTH, CMP_LEN * HEAD_DIM, HEAD_DIM), f32) * (CMP_LEN * HEAD_DIM) ** -0.5
    pool_w = jax.random.normal(ks[8], (DEPTH, POOL_GROUPS, POOL_CH, POOL_CH), f32) * POOL_CH ** -0.5
    pool_scale = 1.0 + 0.1 * jax.random.normal(ks[9], (DEPTH, W_GROUP), f32)
    final_norm_w = 1.0 + 0.1 * jax.random.normal(ks[10], (D_MODEL,), f32)
    return {"x": x, "norm_w": norm_w, "w_in": w_in, "w_out": w_out, "conv_w": conv_w,
            "pe_cmp": pe_cmp, "w_cmp_k": w_cmp_k, "w_cmp_v": w_cmp_v, "pool_w": pool_w,
            "pool_scale": pool_scale, "final_norm_w": final_norm_w}


def reference(x, norm_w, w_in, w_out, conv_w, pe_cmp, w_cmp_k, w_cmp_v, pool_w, pool_scale, final_norm_w):
    for l in range(DEPTH):
        x = hybrid_layer(x, norm_w[l], w_in[l], w_out[l], conv_w[l], pe_cmp[l],
                         w_cmp_k[l], w_cmp_v[l], pool_w[l], pool_scale[l])
    return rmsnorm(x, final_norm_w)
```

```python
import contextlib
import numpy as np
import ml_dtypes
import concourse.bass as bass
import concourse.mybir as mybir
from concourse.bass_utils import run_bass_kernel_spmd

F32 = mybir.dt.float32
BF16 = mybir.dt.bfloat16
F16 = mybir.dt.float16
ALU = mybir.AluOpType
AF = mybir.ActivationFunctionType
AX = mybir.AxisListType

T = 4096
D = 1024
NT = 32
DEPTH = 4
NEG = -30000.0
BIG = 1.0e30
TOPK = 256
NBIS = 9

O_R16, O_IDX, O_KCVC, O_DU, O_V, O_WI, O_G = 0, 896, 1280, 1408, 1664, 1856, 1864
TMW = 1876
O_FM = TMW
WCOLS = TMW + 14 * 128
PIECES = [(0, 1024), (1024, 1024), (2048, 1024), (3072, WCOLS - 3072)]
PK_QA, PK_QB, PK_QI, PK_G = 0, 512, 1024, 2048
PKW = 3072
SP_NW, SP_CONV, SP_PE, SP_PS, SP_POOLW, SPW = 0, 8, 14, 46, 48, 560
CF_COS16, CF_SIN16, CF_COS8, CF_SIN8 = 0, 256, 512, 640
CF_CC, CF_SC, CF_PROT, CF_KEEP, CF_ADD, CF_RM, CF_CAUS = 768, 1024, 1280, 1408, 1536, 1664, 1672
CF_POW = 1800
CFW = 1816
CB_ID, CB_I4, CB_CAUS, CB_WIN4, CB_P8, CB_BAND, CB_OVL = 0, 128, 640, 768, 896, 904, 2440
CB_MQ, CB_MI = 2568, 2824
CBW = 3336


def _in_offsets():
    names = [("a_q", 256), ("a_k", 64), ("a_v", 64), ("a_qi", 256), ("a_ki", 32), ("a_wi", 8), ("a_gate", 256),
             ("b_q", 256), ("b_kc", 64), ("b_vc", 64), ("b_ks", 64), ("b_vs", 64), ("b_kw", 64), ("b_vw", 64),
             ("b_g", 12), ("b_gate", 256), ("c_b", 256), ("c_c", 256), ("c_x", 256), ("c_gate", 256),
             ("d_u", 256), ("d_gate", 256)]
    o = {}
    c = 0
    for n, w in names:
        o[n] = (c, w)
        c += w
    return o


def _colidx():
    o = _in_offsets()
    r = lambda n, a=0, b=None: list(range(o[n][0] + a, o[n][0] + (o[n][1] if b is None else b)))
    idx = []
    idx += r("a_q")
    idx += r("a_k") + r("a_k")
    idx += r("b_q")
    idx += r("b_ks") + r("b_ks")
    idx += r("b_kw") + r("b_kw")
    idx += r("a_qi")
    idx += r("a_ki") * 4
    idx += r("b_kc") + r("b_vc")
    idx += r("d_u")
    idx += r("a_v") + r("b_vs") + r("b_vw")
    idx += r("a_wi") + r("b_g")
    assert len(idx) == TMW
    for n in ["a_gate", "b_gate", "c_b", "c_c", "c_x", "c_gate", "d_gate"]:
        idx += r(n)
    assert len(idx) == WCOLS
    return np.array(idx, dtype=np.int64)


def _consts():
    cf = np.zeros((128, CFW), np.float32)
    pos = np.arange(T, dtype=np.float32)
    for (half, oc, os_) in [(8, CF_COS16, CF_SIN16), (4, CF_COS8, CF_SIN8)]:
        inv = (np.float32(500000.0) ** (-(np.arange(half, dtype=np.float32) / np.float32(half)))).astype(np.float32)
        ang = (pos[:, None] * inv[None, :]).astype(np.float32)
        c = np.cos(ang.astype(np.float64)).astype(np.float32).reshape(NT, 128, half).transpose(1, 0, 2).reshape(128, NT * half)
        s = np.sin(ang.astype(np.float64)).astype(np.float32).reshape(NT, 128, half).transpose(1, 0, 2).reshape(128, NT * half)
        cf[:, oc:oc + NT * half] = c
        cf[:, os_:os_ + NT * half] = s
    inv = (np.float32(500000.0) ** (-(np.arange(8, dtype=np.float32) / np.float32(8)))).astype(np.float32)
    cpos = (np.arange(255) * 16 + 31).astype(np.float32)
    ang = (cpos[:, None] * inv[None, :]).astype(np.float32).astype(np.float64)
    cc = np.ones((64, 256), np.float32)
    sc = np.zeros((64, 256), np.float32)
    for e in range(16):
        cc[e, :255] = np.cos(ang[:, e % 8])
        sc[e, :255] = np.sin(ang[:, e % 8])
    cf[:, CF_CC:CF_CC + 256] = np.tile(cc, (2, 1))
    cf[:, CF_SC:CF_SC + 256] = np.tile(sc, (2, 1))
    prot = np.zeros((64, 64), np.float32)
    for e in range(8):
        prot[e + 8, e] = -1.0
        prot[e, e + 8] = 1.0
    p128 = np.zeros((128, 128), np.float32)
    p128[:64, :64] = prot
    p128[64:, 64:] = prot
    cf[:, CF_PROT:CF_PROT + 128] = p128
    keep = np.zeros((128, 128), np.float32)
    add = np.zeros((128, 128), np.float32)
    for q in range(128):
        cur = 0 if q < 64 else 1
        for col in range(126):
            r = col - 62
            if r < cur:
                keep[q, col] = 1.0
            elif r == cur:
                add[q, col] = BIG
            else:
                add[q, col] = -BIG
    cf[:, CF_KEEP:CF_KEEP + 128] = keep
    cf[:, CF_ADD:CF_ADD + 128] = add
    p = np.arange(128)
    cf[:, CF_RM + 0] = (p < 64)
    cf[:, CF_RM + 1] = (p >= 64)
    for h in range(4):
        cf[:, CF_RM + 2 + h] = (p // 32 == h)
    qq, ss = np.meshgrid(np.arange(128), np.arange(128), indexing="ij")
    cf[:, CF_CAUS:CF_CAUS + 128] = np.where(ss <= qq, 0.0, -BIG)
    for k in range(16):
        cf[:, CF_POW + k] = 2.0 ** (-(k + 1))

    cb = np.zeros((128, CBW), np.float32)
    cb[:, CB_ID:CB_ID + 128] = np.eye(128)
    cb[:, CB_I4:CB_I4 + 512] = np.tile(np.eye(128), (1, 4))
    cb[:, CB_CAUS:CB_CAUS + 128] = np.where(ss <= qq, 0.0, NEG)
    cb[:, CB_WIN4:CB_WIN4 + 128] = np.where(ss > qq, 0.0, NEG)
    for c in range(8):
        m = c - 1
        cb[:, CB_P8 + c] = np.where(np.arange(128) >= 16 * m + 31, 0.0, NEG)
    sl, tl = np.meshgrid(np.arange(128), np.arange(128), indexing="ij")
    for g, w in enumerate((2, 4, 8, 16)):
        cur = ((sl >= tl - w + 1) & (sl <= tl)) / float(w) - (sl == tl)
        prev = ((sl - 128) >= (tl - w + 1)) / float(w)
        cnt0 = np.minimum(tl + 1, w).astype(np.float64)
        cur0 = ((sl >= tl - w + 1) & (sl <= tl)) / cnt0 - (sl == tl)
        for k, m in enumerate((cur, prev, cur0)):
            o = CB_BAND + (g * 3 + k) * 128
            cb[:, o:o + 128] = m
    n = np.arange(256)
    for j in range(1, 64):
        ov = ((16 * n < 64 * j + 64) & (16 * n + 31 >= 64 * j) & (n < 255)).astype(np.float32)
        cb[:, CB_OVL + (j - 1)] = ov[:128]
        cb[:, CB_OVL + 64 + (j - 1)] = ov[128:]
    cb[:, CB_MQ:CB_MQ + 128] = (p < 64)[:, None]
    cb[:, CB_MQ + 128:CB_MQ + 256] = (p >= 64)[:, None]
    for h in range(4):
        cb[:, CB_MI + h * 128:CB_MI + (h + 1) * 128] = (p // 32 == h)[:, None]
    return cf, cb.astype(ml_dtypes.bfloat16)


class Region:
    __slots__ = ("name", "w", "r")

    def __init__(self, name):
        self.name = name
        self.w = None
        self.r = {}


ENGS = ("pe", "act", "dve", "pool", "sp")


class _Rec:
    def __init__(self):
        self.call = None

    def __getattr__(self, name):
        def f(*a, **k):
            self.call = (name, a, k)
            return None
        return f


class Prog:
    def __init__(self):
        self.ops = {e: [] for e in ENGS}
        self.cnt = {e: 0 for e in ENGS}
        self.known = {e: {} for e in ENGS}
        self.dma_cnt = {}
        self.waited = {e: set() for e in ENGS}

    def emit(self, eng, fn, r=(), w=(), slot=None):
        d = {}

        def add(ev, raw):
            if ev is None:
                return
            k, v = ev
            if k == eng and eng == "pe":
                return
            if d.get(k, 0) < v:
                d[k] = v
        for R in r:
            add(R.w, True)
        for R in w:
            add(R.w, False)
            for k, v in R.r.items():
                add((k, v), False)
        kn = self.known[eng]
        for k, v in d.items():
            if kn.get(k, 0) < v:
                kn[k] = v
                self.ops[eng].append(("wait", k, v))
                if k in self.waited:
                    self.waited[k].add(v)
        rec = _Rec()
        fn(rec)
        fn = rec.call
        assert fn is not None
        if slot is None:
            self.cnt[eng] += 1
            ev = (eng, self.cnt[eng])
            self.ops[eng].append(("op", fn, self.cnt[eng]))
        else:
            c = self.dma_cnt.get(slot, 0) + 16
            self.dma_cnt[slot] = c
            ev = (slot, c)
            self.ops[eng].append(("dma", fn, slot))
        for R in r:
            if R.r.get(ev[0], 0) < ev[1]:
                R.r[ev[0]] = ev[1]
        for R in w:
            R.w = ev
            R.r = {}
        return ev

    def barrier(self):
        targets = {e: self.cnt[e] for e in ENGS if self.cnt[e] > 0}
        for s, c in self.dma_cnt.items():
            targets[s] = c
        for eng in ENGS:
            kn = self.known[eng]
            for k, v in targets.items():
                if k == eng:
                    continue
                if kn.get(k, 0) < v:
                    kn[k] = v
                    self.ops[eng].append(("wait", k, v))
                    if k in self.waited:
                        self.waited[k].add(v)

    def build(self, nc, es):
        sems = {e: es.enter_context(nc.semaphore("sem_" + e)) for e in ENGS}
        for s in self.dma_cnt:
            sems[s] = es.enter_context(nc.semaphore("dsem_" + s))
        rank = {}
        for e in ENGS:
            rank[e] = {v: i + 1 for i, v in enumerate(sorted(self.waited[e]))}

        def replay(handle, e):
            for op in self.ops[e]:
                if op[0] == "wait":
                    k, v = op[1], op[2]
                    val = rank[k][v] if k in rank else v
                    handle.wait_ge(sems[k], val)
                elif op[0] == "op":
                    name, a, k = op[1]
                    ins = getattr(handle, name)(*a, **k)
                    if op[2] in rank[e]:
                        ins.then_inc(sems[e], 1)
                else:
                    name, a, k = op[1]
                    getattr(handle, name)(*a, **k).then_inc(sems[op[2]], 16)

        with nc.Block() as block:
            @block.tensor
            def _(h):
                replay(h, "pe")

            @block.scalar
            def _(h):
                replay(h, "act")

            @block.vector
            def _(h):
                replay(h, "dve")

            @block.gpsimd
            def _(h):
                replay(h, "pool")

            @block.sync
            def _(h):
                replay(h, "sp")


def r3(ap, b):
    return ap.rearrange("p (a b) -> p a b", b=b)


def build_program(n_layers=DEPTH, final_norm=True, dbg_pack=False, nt_run=NT, stop_after=None, p1_cut=None):
    nc = bass.Bass("TRN2", target_bir_lowering=False)
    P = Prog()
    es = contextlib.ExitStack()
    dram = lambda n, s, dt, kind: nc.dram_tensor(n, s, dt, kind=kind).ap()
    x_in = dram("x", [T, D], F32, "ExternalInput")
    w_in_d = dram("w_in", [DEPTH, D, WCOLS], F32, "ExternalInput")
    w_out_d = dram("w_out", [DEPTH, D, D], F32, "ExternalInput")
    wcmp_d = dram("wcmp", [DEPTH, 128, 4096], F32, "ExternalInput")
    smallp_d = dram("smallp", [DEPTH, 128, SPW], F32, "ExternalInput")
    fnw_d = dram("fnw", [1, D], F32, "ExternalInput")
    cf_d = dram("cst_f", [128, CFW], F32, "ExternalInput")
    cb_d = dram("cst_b", [128, CBW], BF16, "ExternalInput")
    out_d = dram("out", [T, D], F32, "ExternalOutput")
    pack_d = dram("pack_d", [NT, 128, PKW], BF16, "ExternalOutput" if dbg_pack else "Internal")
    xs_d = [dram("xs0", [T, D], F32, "Internal"), dram("xs1", [T, D], F32, "Internal")]
    R_packd = [Region("packd%d" % i) for i in range(NT)]
    R_xs = [[Region("xs%d_%d" % (k, i)) for i in range(NT)] for k in range(2)]

    def sb(name, shape, dt):
        return es.enter_context(nc.sbuf_tensor("s_" + name, shape, dt))

    arena1 = sb("arena1", [128, 8 * WCOLS], BF16)
    arena2 = sb("arena2", [128, 7936], F32)
    w_out = sb("w_out", [128, 8, D], BF16)
    kaT = sb("kaT", [128, T], BF16)
    ksT = sb("ksT", [128, T], BF16)
    kwT = sb("kwT", [128, T], BF16)
    kiT = sb("kiT", [128, T], BF16)
    V_all = sb("V_all", [128, NT, 3, 65], BF16)
    xt = [sb("xt%d" % i, [128, D], F32) for i in range(2)]
    hb2 = [sb("hb%d" % i, [128, D], BF16) for i in range(2)]
    hT2 = [sb("hT%d" % i, [128, 8, 128], BF16) for i in range(2)]
    hb = hb2[0]
    cf = sb("cf", [128, CFW], F32)
    cb = sb("cb", [128, CBW], BF16)
    smallp = sb("smallp", [128, SPW], F32)
    poolw = sb("poolw", [128, 512], BF16)
    fnw = sb("fnw", [128, D], F32)
    wi_all = sb("wi_all", [128, NT, 8], F32)
    g_all = sb("g_all", [128, NT, 12], F32)
    kcmpT = sb("kcmpT", [128, 256], BF16)
    vcmp = sb("vcmp", [128, 2, 128], BF16)
    maskC = sb("maskC", [128, 264], BF16)
    du = [sb("du%d" % i, [128, 256], BF16) for i in range(2)]
    uext = sb("uext", [128, 2, 130], F32)
    ccs = sb("ccs", [128, 256], F32)
    sgc = sb("sgc", [128, 256], F32)
    ysb = sb("ysb", [128, 2, 128], F32)
    tcb = sb("tcb", [128, 256], F32)
    sgd = sb("sgd", [128, 256], F32)
    pooled = sb("pooled", [128, 512], BF16)
    rt = sb("rt", [128, 4, 112], F32)
    rt2 = sb("rt2", [128, 4, 48], F32)
    sm = sb("sm", [128, 128], F32)
    imp = sb("imp", [128, 64], F32)
    imp2 = sb("imp2", [128, 64], F32)
    imp3 = sb("imp3", [128, 64], F32)
    ps = [es.enter_context(nc.psum_tensor("ps%d" % i, [128, 512], F32)) for i in range(8)]
    psb = [p_[:, :].bitcast(BF16) for p_ in ps]
    R_ps = [Region("ps%d" % i) for i in range(8)]

    w_in = r3(arena1[:, :], WCOLS)
    score = arena1[:, 0:4096].bitcast(F16)
    maskA2 = [arena1[:, 8192:12288], arena1[:, 20480:24576]]
    maskB2 = [arena1[:, 12288:16384], arena1[:, 24576:28672]]
    junk = arena1[:, 16384:20480]
    wcmp = r3(arena1[:, 16384:20480], 128)
    kcvc_tmp = [arena1[:, 20480 + 256 * i: 20480 + 256 * (i + 1)] for i in range(3)]
    kc_sb = arena1[:, 21504:22016].bitcast(F32)
    kc_t1 = arena1[:, 22016:22528].bitcast(F32)
    kc_t2 = arena1[:, 22528:23040].bitcast(F32)
    a2 = arena2
    wst = [a2[:, 1024 * i:1024 * (i + 1)] for i in range(4)]
    zs = a2[:, 0:TMW]
    zb = a2[:, 1880:1880 + 704].bitcast(BF16)
    packt = [a2[:, 2600 + 1536 * i: 2600 + 1536 * (i + 1)].bitcast(BF16) for i in range(2)]
    kcvcT = a2[:, 5680:5680 + 2048].bitcast(BF16)
    pk = [a2[:, 1024 * i:1024 * (i + 1)].bitcast(BF16) for i in range(2)]
    pkg = [a2[:, 2048 + 512 * i:2048 + 512 * (i + 1)].bitcast(BF16) for i in range(3)]
    Rt = [a2[:, 3584 + 256 * k:3584 + 256 * (k + 1)].bitcast(BF16) for k in range(3)] + [a2[:, 7680:7936].bitcast(BF16)]
    pT = [a2[:, 4352 + 256 * k:4352 + 256 * (k + 1)].bitcast(BF16) for k in range(3)]
    diagW = r3(a2[:, 5120:5632].bitcast(BF16), 128)
    oa_bf = a2[:, 5632:5760].bitcast(BF16)
    ob_bf = a2[:, 5760:5888].bitcast(BF16)
    ob32 = a2[:, 5888:6144]
    mx = a2[:, 6144:6400].bitcast(BF16)
    xn = a2[:, 6400:7424]
    tmp32 = a2[:, 7424:7680]

    ident = cb[:, CB_ID:CB_ID + 128]
    I4 = cb[:, CB_I4:CB_I4 + 512]
    causb = cb[:, CB_CAUS:CB_CAUS + 128]
    win4b = cb[:, CB_WIN4:CB_WIN4 + 128]

    SS, MS, SD, RSTD = sm[:, 0:1], sm[:, 1:2], sm[:, 2:3], sm[:, 3:4]
    HI, LO, W0 = sm[:, 4:5], sm[:, 5:6], sm[:, 6:7]
    WK = sm[:, 8:24]
    TB = [sm[:, 24:25], sm[:, 25:26]]
    CNT, UU = sm[:, 26:27], sm[:, 27:28]
    RDA, DENC, RDC, FC = sm[:, 32:36], sm[:, 36:40], sm[:, 40:44], sm[:, 44:48]
    RDS, FS, RDW, FW = sm[:, 48:52], sm[:, 52:56], sm[:, 56:60], sm[:, 60:64]
    M8A, M8B = sm[:, 64:72], sm[:, 72:80]
    R_sm = {k: Region("sm_" + k) for k in ["ss", "ms", "sd", "rstd", "hi", "lo", "w0", "wk", "t0", "t1", "cnt", "uu",
                                            "rda", "denc", "rdc", "fc", "rds", "fs", "rdw", "fw", "m8a", "m8b"]}

    R = {k: Region(k) for k in ["hb", "hT", "zs", "zb", "rt", "uext", "ccs", "sgc", "ysb", "tcb", "sgd", "pooled",
                                "smallp", "poolw", "score", "maskA", "maskB", "maskC", "diagW", "oa", "ob", "ob32",
                                "mx", "xn", "tmp32", "imp", "imp2", "imp3", "kcmpT", "vcmp", "kc_sb", "kc_t1", "kc_t2",
                                "wcmp", "consts"]}
    R_xt = [Region("xt0"), Region("xt1")]
    R["rt2"] = Region("rt2")
    R["zbA"] = Region("zbA")
    R["zbB"] = Region("zbB")
    R_hb = [Region("hb0"), Region("hb1")]
    R_hT = [Region("hT0"), Region("hT1")]
    R_du = [Region("du0"), Region("du1")]
    R_wst = [Region("wst%d" % i) for i in range(4)]
    R_pkt = [{k: Region("pkt%d_%s" % (i, k)) for k in ["qa", "qb", "qi", "gab", "mc", "md"]} for i in range(2)]
    R_pk = [Region("pk0"), Region("pk1")]
    R_pkg = [Region("pkg%d" % i) for i in range(3)]
    R_Rt = [Region("Rt%d" % i) for i in range(4)]
    R_pT = [Region("pT%d" % i) for i in range(3)]
    R_tmpj = [Region("tmpj%d" % i) for i in range(3)]
    R_mA = [Region("maskA0"), Region("maskA1")]
    R_mB = [Region("maskB0"), Region("maskB1")]
    R["junk"] = Region("junk")

    act, dve, pool, pe = (lambda f, r=(), w=(): P.emit("act", f, r, w)), (lambda f, r=(), w=(): P.emit("dve", f, r, w)), \
        (lambda f, r=(), w=(): P.emit("pool", f, r, w)), (lambda f, r=(), w=(): P.emit("pe", f, r, w))

    def dma(f, r, w, slot, q="sp"):
        P.emit(q, f, r, w, slot=slot)

    dma(lambda e: e.dma_start(out=cf[:, :], in_=cf_d[:, :]), (), (R["consts"],), "c0")
    dma(lambda e: e.dma_start(out=cb[:, :], in_=cb_d[:, :]), (), (R["consts"],), "c1")
    dma(lambda e: e.dma_start(out=fnw[:, :], in_=fnw_d[0:1, :].broadcast_to([128, D])), (), (R["consts"],), "c2")
    pool(lambda e: e.memset(V_all[:, :, :, 64:65], 1.0))
    pool(lambda e: e.memset(kcmpT[:, :], 0.0), (), (R["kcmpT"],))
    pool(lambda e: e.memset(vcmp[:, :, :], 0.0), (), (R["vcmp"],))
    pool(lambda e: e.memset(vcmp[:, :, 64:65], 1.0), (), (R["vcmp"],))
    pool(lambda e: e.tensor_copy(vcmp[:, :, 65:128], r3(cb[:, CB_OVL:CB_OVL + 128], 64)[:, :, 0:63]), (R["consts"],), (R["vcmp"],))
    pool(lambda e: e.memset(imp[:, :], BIG), (), (R["imp"],))
    P.barrier()

    cast_rr = [0]

    def cast(out_ap, in_ap, scale_ap, r, w):
        k = cast_rr[0] % 3
        cast_rr[0] += 1
        if k == 0:
            if scale_ap is None:
                pool(lambda e: e.tensor_copy(out_ap, in_ap), r, w)
            else:
                pool(lambda e: e.tensor_scalar(out_ap, in_ap, scale_ap, None, ALU.mult), r, w)
        elif k == 1:
            if scale_ap is None:
                dve(lambda e: e.tensor_copy(out_ap, in_ap), r, w)
            else:
                dve(lambda e: e.tensor_scalar(out_ap, in_ap, scale_ap, None, ALU.mult), r, w)
        else:
            if scale_ap is None:
                act(lambda e: e.copy(out_ap, in_ap), r, w)
            else:
                act(lambda e: e.activation(out_ap, in_ap, AF.Identity, scale=scale_ap), r, w)

    sbank = [0]
    ibank = [0]
    obank = [0]
    rti = [0]
    pti = [0]

    def rmsnorm_stats(src_ap, r_src):
        act(lambda e: e.activation(hb[:, :], src_ap, AF.Square, accum_out=SS), (r_src,), (R["hb"], R_sm["ss"]))
        dve(lambda e: e.tensor_scalar(MS, SS, 1.0 / D, 1e-6, ALU.mult, ALU.add), (R_sm["ss"],), (R_sm["ms"],))
        act(lambda e: e.activation(SD, MS, AF.Sqrt), (R_sm["ms"],), (R_sm["sd"],))
        dve(lambda e: e.reciprocal(RSTD, SD), (R_sm["sd"],), (R_sm["rstd"],))

    for l in range(n_layers):
        xsrc = x_in if l == 0 else xs_d[(l - 1) % 2]
        R_xsrc = None if l == 0 else R_xs[(l - 1) % 2]
        last = (l == n_layers - 1)
        dma(lambda e: e.dma_start(out=smallp[:, :], in_=smallp_d[l, :, :]), (), (R["smallp"],), "smallp")
        pool(lambda e: e.tensor_copy(poolw[:, :], smallp[:, SP_POOLW:SP_POOLW + 512]), (R["smallp"],), (R["poolw"],))
        k = 0
        for kc in range(8):
            for (c0, w) in PIECES:
                s_ = k % 4
                k += 1
                dma(lambda e, s_=s_, kc=kc, c0=c0, w=w: e.dma_start(out=wst[s_][:, 0:w], in_=w_in_d[l, kc * 128:(kc + 1) * 128, c0:c0 + w]),
                    (), (R_wst[s_],), "wst%d" % s_, q=("sp" if k % 2 else "act"))
                cast(w_in[:, kc, c0:c0 + w], wst[s_][:, 0:w], smallp[:, SP_NW + kc:SP_NW + kc + 1], (R_wst[s_], R["smallp"]), ())
        for kc in range(8):
            s_ = k % 4
            k += 1
            dma(lambda e, s_=s_, kc=kc: e.dma_start(out=wst[s_][:, :], in_=w_out_d[l, kc * 128:(kc + 1) * 128, :]), (), (R_wst[s_],), "wst%d" % s_)
            cast(w_out[:, kc, :], wst[s_][:, :], None, (R_wst[s_],), ())
        pool(lambda e: e.memset(uext[:, :, 0:2], 0.0), (), (R["uext"],))
        pool(lambda e: e.memset(maskC[:, :], NEG), (), (R["maskC"],))
        P.barrier()
        if stop_after == "p0":
            break
        def norm_part(tt):
            s_ = tt % 2
            dma(lambda e: e.dma_start(out=xt[s_][:, :], in_=xsrc[tt * 128:(tt + 1) * 128, :]),
                () if R_xsrc is None else (R_xsrc[tt],), (R_xt[s_],), "xt%d" % s_)
            act(lambda e: e.activation(hb2[s_][:, :], xt[s_][:, :], AF.Square, accum_out=SS), (R_xt[s_],), (R_hb[s_], R_sm["ss"]))
            dve(lambda e: e.tensor_scalar(MS, SS, 1.0 / D, 1e-6, ALU.mult, ALU.add), (R_sm["ss"],), (R_sm["ms"],))
            act(lambda e: e.activation(SD, MS, AF.Sqrt), (R_sm["ms"],), (R_sm["sd"],))
            dve(lambda e: e.reciprocal(RSTD, SD), (R_sm["sd"],), (R_sm["rstd"],))
            act(lambda e: e.activation(hb2[s_][:, :], xt[s_][:, :], AF.Identity, scale=RSTD), (R_xt[s_], R_sm["rstd"]), (R_hb[s_],))
            for kc in range(8):
                pe(lambda e, kc=kc: e.transpose(psb[0][:, kc * 128:(kc + 1) * 128], hb2[s_][:, kc * 128:(kc + 1) * 128], ident),
                   (R_hb[s_], R["consts"]), (R_ps[0],))
            dve(lambda e: e.tensor_copy(hT2[s_][:, :, :], r3(psb[0][:, 0:1024], 128)), (R_ps[0],), (R_hT[s_],))

        norm_part(0)
        for tt in range(nt_run):
            s_ = tt % 2
            pkt = packt[s_]
            Rk = R_pkt[s_]
            hT = hT2[s_]
            R["hT"] = R_hT[s_]
            for ci, (c0, w) in enumerate([(0, 512), (512, 512), (1024, 512), (1536, TMW - 1536)]):
                bk = 1 + ci % 2
                for kc in range(8):
                    pe(lambda e, bk=bk, kc=kc, c0=c0, w=w: e.matmul(ps[bk][:, 0:w], hT[:, kc, :], w_in[:, kc, c0:c0 + w], start=(kc == 0), stop=(kc == 7)),
                       (R["hT"],), (R_ps[bk],))
                act(lambda e, bk=bk, c0=c0, w=w: e.copy(zs[:, c0:c0 + w], ps[bk][:, 0:w]), (R_ps[bk],), (R["zs"],))

            if tt + 1 < nt_run:
                norm_part(tt + 1)
            def fm_group(bk, blocks):
                for bi, blk in enumerate(blocks):
                    for kc in range(8):
                        pe(lambda e, bi=bi, blk=blk, kc=kc: e.matmul(ps[bk][:, bi * 128:(bi + 1) * 128], w_in[:, kc, O_FM + blk * 128:O_FM + (blk + 1) * 128],
                                                                      hT[:, kc, :], start=(bi == 0 and kc == 0), stop=(kc == 7), skip_group_check=True),
                           (R["hT"],), (R_ps[bk],))
            fm_group(3, [0, 1, 2, 3])
            act(lambda e, pkt=pkt: e.activation(pkt[:, PK_G:PK_G + 512], ps[3][:, 0:512], AF.Silu), (R_ps[3],), (Rk["gab"],))
            fm_group(4, [6, 7, 8, 9])
            act(lambda e: e.copy(ccs[:, :], ps[4][:, 0:256]), (R_ps[4],), (R["ccs"],))
            if tt > 0:
                dve(lambda e: e.tensor_copy(uext[:, :, 0:2], uext[:, :, 128:130]), (R["uext"],), (R["uext"],))
            dve(lambda e: e.tensor_tensor(uext[:, :, 2:130], r3(ps[4][:, 256:512], 128), r3(ccs[:, :], 128), ALU.mult),
                (R_ps[4], R["ccs"]), (R["uext"],))
            fm_group(3, [4, 5, 10, 11])
            act(lambda e: e.activation(sgc[:, :], ps[3][:, 256:512], AF.Silu), (R_ps[3],), (R["sgc"],))
            for j in range(2):
                cw = lambda q, j=j: smallp[:, SP_CONV + j * 3 + q:SP_CONV + j * 3 + q + 1]
                dve(lambda e, j=j, cw=cw: e.tensor_scalar(ysb[:, j, :], uext[:, j, 0:128], cw(0), None, ALU.mult), (R["uext"], R["smallp"]), (R["ysb"],))
                dve(lambda e, j=j, cw=cw: e.scalar_tensor_tensor(ysb[:, j, :], uext[:, j, 1:129], cw(1), ysb[:, j, :], ALU.mult, ALU.add), (R["uext"], R["ysb"]), (R["ysb"],))
                dve(lambda e, j=j, cw=cw: e.scalar_tensor_tensor(ysb[:, j, :], uext[:, j, 2:130], cw(2), ysb[:, j, :], ALU.mult, ALU.add), (R["uext"], R["ysb"]), (R["ysb"],))
            dve(lambda e: e.tensor_tensor(tcb[:, :], ps[3][:, 0:256], ysb[:, :, :].rearrange("p a b -> p (a b)"), ALU.mult), (R_ps[3], R["ysb"]), (R["tcb"],))
            pool(lambda e, pkt=pkt: e.tensor_tensor(pkt[:, PK_G + 512:PK_G + 768], tcb[:, :], sgc[:, :], ALU.mult), (R["tcb"], R["sgc"]), (Rk["mc"],))
            cs16 = lambda o, tt=tt: cf[:, o + tt * 8:o + tt * 8 + 8].unsqueeze(1).broadcast_to([128, 14, 8])
            cs8 = lambda o, tt=tt: cf[:, o + tt * 4:o + tt * 4 + 4].unsqueeze(1).broadcast_to([128, 12, 4])
            dve(lambda e, s_=s_: e.tensor_copy(du[s_][:, :], zs[:, O_DU:O_DU + 256]), (R["zs"],), (R_du[s_],))
            dve(lambda e, tt=tt: e.tensor_copy(V_all[:, tt, :, 0:64], r3(zs[:, O_V:O_V + 192], 64)), (R["zs"],), ())
            dve(lambda e, tt=tt: e.tensor_copy(wi_all[:, tt, :], zs[:, O_WI:O_WI + 8]), (R["zs"],), ())
            for (zin, zout, nh, hd, half, cs, oc, os_, eng, rtb, rtk, zk) in [
                (zs[:, 0:896], zb[:, 0:896], 14, 64, 8, cs16, CF_COS16, CF_SIN16, pool, rt, "rt", "zbA"),
                (zs[:, 896:1280], zb[:, 896:1280], 12, 32, 4, cs8, CF_COS8, CF_SIN8, dve, rt2, "rt2", "zbB")]:
                zi = r3(zin, hd)
                zo = r3(zout, hd)
                x1, x2 = zi[:, :, 0:half], zi[:, :, half:2 * half]
                n = nh * half
                t_ = [rtb[:, q, 0:n].rearrange("p (a b) -> p a b", b=half) for q in range(4)]
                rr, ww = (R["zs"], R["consts"]), (R[rtk],)
                eng(lambda e: e.tensor_tensor(t_[0], x1, cs(oc), ALU.mult), rr, ww)
                eng(lambda e: e.tensor_tensor(t_[1], x2, cs(os_), ALU.mult), rr, ww)
                eng(lambda e: e.tensor_tensor(t_[2], x2, cs(oc), ALU.mult), rr, ww)
                eng(lambda e: e.tensor_tensor(t_[3], x1, cs(os_), ALU.mult), rr, ww)
                eng(lambda e: e.tensor_tensor(zo[:, :, 0:half], t_[0], t_[1], ALU.subtract), (R[rtk],), (R[zk],))
                eng(lambda e: e.tensor_tensor(zo[:, :, half:2 * half], t_[2], t_[3], ALU.add), (R[rtk],), (R[zk],))
                eng(lambda e: e.tensor_copy(zo[:, :, 2 * half:hd], zi[:, :, 2 * half:hd]), (R["zs"],), (R[zk],))
            dve(lambda e: e.tensor_copy(zb[:, 1280:1408], zs[:, 1280:1408]), (R["zs"],), (R["zbB"],))
            act(lambda e, tt=tt: e.activation(g_all[:, tt, :], zs[:, O_G:O_G + 12], AF.Sigmoid), (R["zs"],), ())
            fm_group(4, [12, 13])
            act(lambda e: e.activation(sgd[:, :], ps[4][:, 0:256], AF.Silu), (R_ps[4],), (R["sgd"],))
            for g in range(4):
                bo = CB_BAND + (g * 3 + (2 if tt == 0 else 0)) * 128
                pe(lambda e, g=g, bo=bo, s_=s_: e.matmul(ps[5][0:64, g * 128:(g + 1) * 128], du[s_][:, g * 64:(g + 1) * 64], cb[:, bo:bo + 128],
                                                          start=(g == 0), stop=(tt == 0), skip_group_check=True), (R_du[s_], R["consts"]), (R_ps[5],))
                if tt > 0:
                    bo2 = CB_BAND + (g * 3 + 1) * 128
                    pe(lambda e, g=g, bo2=bo2, s_=s_: e.matmul(ps[5][0:64, g * 128:(g + 1) * 128], du[1 - s_][:, g * 64:(g + 1) * 64], cb[:, bo2:bo2 + 128],
                                                                start=False, stop=True, skip_group_check=True), (R_du[1 - s_], R["consts"]), (R_ps[5],))
            act(lambda e: e.copy(pooled[0:64, :], ps[5][0:64, :]), (R_ps[5],), (R["pooled"],))
            for j in range(2):
                for gg in range(2):
                    g = 2 * j + gg
                    pe(lambda e, j=j, g=g, gg=gg: e.matmul(ps[4][:, 256 + j * 128:256 + (j + 1) * 128], poolw[0:64, g * 128:(g + 1) * 128], pooled[0:64, g * 128:(g + 1) * 128],
                                                            start=False, stop=(gg == 1), skip_group_check=True), (R["pooled"], R["poolw"]), (R_ps[4],))
            for j in range(2):
                dve(lambda e, j=j, pkt=pkt: e.scalar_tensor_tensor(pkt[:, PK_G + (6 + j) * 128:PK_G + (7 + j) * 128], ps[4][:, 256 + j * 128:256 + (j + 1) * 128],
                                                                   smallp[:, SP_PS + j:SP_PS + j + 1], sgd[:, j * 128:(j + 1) * 128], ALU.mult, ALU.mult),
                    (R_ps[4], R["sgd"], R["smallp"]), (Rk["md"],))
            for b in range(11):
                bk, off = (6, b * 128) if b < 8 else (7, (b - 8) * 128)
                pe(lambda e, b=b, bk=bk, off=off: e.transpose(psb[bk][:, off:off + 128], zb[:, b * 128:(b + 1) * 128], ident), (R["zbA" if b < 7 else "zbB"], R["consts"]), (R_ps[bk],))
            MQ = r3(cb[:, CB_MQ:CB_MQ + 256], 128)
            MI = r3(cb[:, CB_MI:CB_MI + 512], 128)
            bc = lambda ap, n: ap.unsqueeze(1).broadcast_to([128, n, 128])
            for j, (bsrc, dst, key) in enumerate([(0, PK_QA, "qa"), (1, PK_QA + 256, "qa"), (3, PK_QB, "qb"), (4, PK_QB + 256, "qb")]):
                dve(lambda e, bsrc=bsrc, dst=dst, pkt=pkt: e.tensor_tensor(r3(pkt[:, dst:dst + 256], 128), bc(psb[6][:, bsrc * 128:(bsrc + 1) * 128], 2), MQ, ALU.mult),
                    (R_ps[6], R["consts"]), (Rk[key],))
            dve(lambda e, pkt=pkt: e.tensor_tensor(r3(pkt[:, PK_QI:PK_QI + 512], 128), bc(psb[6][:, 7 * 128:8 * 128], 4), MI, ALU.mult), (R_ps[6], R["consts"]), (Rk["qi"],))
            dve(lambda e, pkt=pkt: e.tensor_tensor(r3(pkt[:, PK_QI + 512:PK_QI + 1024], 128), bc(psb[7][:, 0:128], 4), MI, ALU.mult), (R_ps[7], R["consts"]), (Rk["qi"],))
            tsl = slice(tt * 128, (tt + 1) * 128)
            dve(lambda e, tsl=tsl: e.tensor_copy(kaT[:, tsl], psb[6][:, 2 * 128:3 * 128]), (R_ps[6],), ())
            dve(lambda e, tsl=tsl: e.tensor_copy(ksT[:, tsl], psb[6][:, 5 * 128:6 * 128]), (R_ps[6],), ())
            dve(lambda e, tsl=tsl: e.tensor_copy(kwT[:, tsl], psb[6][:, 6 * 128:7 * 128]), (R_ps[6],), ())
            dve(lambda e, tsl=tsl: e.tensor_copy(kiT[:, tsl], psb[7][:, 128:256]), (R_ps[7],), ())
            dve(lambda e, tsl=tsl: e.tensor_copy(kcvcT[:, tsl], psb[7][:, 256:384]), (R_ps[7],), ())
            dma(lambda e, pkt=pkt, tt=tt: e.dma_start(out=pack_d[tt, :, :], in_=pkt[:, :]), tuple(Rk.values()), (R_packd[tt],), "pst%d" % s_)
        if nt_run < NT:
            pool(lambda e: e.memset(kcvcT[:, nt_run * 128:], 0.0))
        P.barrier()
        if stop_after == "p1":
            break
        for c in range(4):
            dma(lambda e, c=c: e.dma_start(out=wst[c][:, :], in_=wcmp_d[l, :, c * 1024:(c + 1) * 1024]), (), (R_wst[c],), "wst%d" % c)
            cast(arena1[:, 16384 + c * 1024:16384 + (c + 1) * 1024], wst[c][:, :], None, (R_wst[c],), (R["wcmp"],))
        for j in range(32):
            tj = kcvc_tmp[j % 3]
            Rj = R_tmpj[j % 3]
            src = kcvcT[:, j:j + 16 * 254 + 1:16]
            pesc = smallp[:, SP_PE + j:SP_PE + j + 1]
            (pool if j % 2 == 0 else dve)(lambda e, tj=tj, src=src, pesc=pesc: e.tensor_scalar(tj[:, 0:255], src, pesc, None, ALU.add), (R["smallp"],), (Rj,))
            pe(lambda e, j=j, tj=tj: e.matmul(ps[0][:, 0:255], wcmp[0:64, j, :], tj[0:64, 0:255], start=(j == 0), stop=(j == 31)), (Rj, R["wcmp"]), (R_ps[0],))
            pe(lambda e, j=j, tj=tj: e.matmul(ps[1][:, 0:64], tj[64:128, 0:128], wcmp[64:128, j, 0:64], start=(j == 0), stop=(j == 31), skip_group_check=True), (Rj, R["wcmp"]), (R_ps[1],))
            pe(lambda e, j=j, tj=tj: e.matmul(ps[1][0:127, 64:128], tj[64:128, 128:255], wcmp[64:128, j, 0:64], start=False, stop=(j == 31), skip_group_check=True), (Rj, R["wcmp"]), (R_ps[1],))
        act(lambda e: e.copy(kc_sb[:, 0:255], ps[0][:, 0:255]), (R_ps[0],), (R["kc_sb"],))
        pe(lambda e: e.matmul(ps[2][:, 0:255], cf[:, CF_PROT:CF_PROT + 128], kc_sb[:, 0:255], start=True, stop=True), (R["kc_sb"], R["consts"]), (R_ps[2],))
        dve(lambda e: e.tensor_tensor(kc_t1[:, 0:255], kc_sb[:, 0:255], cf[:, CF_CC:CF_CC + 255], ALU.mult), (R["kc_sb"],), (R["kc_t1"],))
        dve(lambda e: e.tensor_tensor(kc_t2[:, 0:255], ps[2][:, 0:255], cf[:, CF_SC:CF_SC + 255], ALU.mult), (R_ps[2],), (R["kc_t2"],))
        dve(lambda e: e.tensor_tensor(kcmpT[:, 0:255], kc_t1[:, 0:255], kc_t2[:, 0:255], ALU.add), (R["kc_t1"], R["kc_t2"]), (R["kcmpT"],))
        act(lambda e: e.copy(vcmp[:, 0, 0:64], ps[1][:, 0:64]), (R_ps[1],), (R["vcmp"],))
        act(lambda e: e.copy(vcmp[0:127, 1, 0:64], ps[1][0:127, 64:128]), (R_ps[1],), (R["vcmp"],))
        P.barrier()
        if stop_after == "p15":
            break
        def attn_loop(kts, kT, qoff, vidx, maskfn, pks, Rpk):
            ob = 2 + obank[0] % 2
            obank[0] += 1
            acc = ps[ob]
            nk = len(kts)

            def qk(kt):
                sbk = sbank[0] % 2
                sbank[0] += 1
                ksl = slice(kt * 128, (kt + 1) * 128)
                m = maskfn(kt)
                pe(lambda e: e.matmul(ps[sbk][:, 0:256], kT[:, ksl], pks[:, qoff:qoff + 256], start=True, stop=False, skip_group_check=True), (Rpk,), (R_ps[sbk],))
                pe(lambda e: e.matmul(ps[sbk][:, 256:512], kT[:, ksl], pks[:, qoff + 256:qoff + 512], start=False, stop=(m is None), skip_group_check=True), (Rpk,), (R_ps[sbk],))
                if m is not None:
                    map_, mr = m
                    pe(lambda e: e.matmul(ps[sbk][:, 0:512], map_, I4, start=False, stop=True, skip_group_check=True), (mr, R["consts"]), (R_ps[sbk],))
                return sbk

            nxt = qk(kts[0])
            for n_, kt in enumerate(kts):
                sbk = nxt
                pi = pti[0] % 3
                pti[0] += 1
                act(lambda e: e.activation(pT[pi][:, :], ps[sbk][:, :], AF.Exp, scale=0.125), (R_ps[sbk],), (R_pT[pi],))
                if n_ + 1 < nk:
                    nxt = qk(kts[n_ + 1])
                for h in range(4):
                    pe(lambda e: e.matmul(acc[:, h * 65:(h + 1) * 65], pT[pi][:, h * 128:(h + 1) * 128], V_all[:, kt, vidx, :],
                                          start=(n_ == 0 and h == 0), stop=(n_ == nk - 1), skip_group_check=True), (R_pT[pi],), (R_ps[ob],))
            return ob

        def out_part(i):
            s_ = i % 2
            pgs = pkg[i % 3]
            Rpg = R_pkg[i % 3]
            for b in range(4):
                src = oa_bf if b < 2 else ob_bf
                pe(lambda e: e.transpose(psb[5][:, b * 128:(b + 1) * 128], src[:, (b % 2) * 128:(b % 2 + 1) * 128], ident), (R["oa"], R["ob"], R["consts"]), (R_ps[5],))
            dve(lambda e: e.tensor_tensor(mx[:, :], psb[5][:, 0:512], pgs[:, 0:512], ALU.mult), (R_ps[5], Rpg), (R["mx"],))
            for n in range(2):
                for b in range(8):
                    lh = mx[:, b * 128:(b + 1) * 128] if b < 4 else pgs[:, b * 128:(b + 1) * 128]
                    pe(lambda e: e.matmul(ps[6][:, :], lh, w_out[:, b, n * 512:(n + 1) * 512], start=(b == 0), stop=(b == 7)), (R["mx"], Rpg), (R_ps[6],))
                dve(lambda e: e.tensor_tensor(xn[:, n * 512:(n + 1) * 512], ps[6][:, :], xt[s_][:, n * 512:(n + 1) * 512], ALU.add), (R_ps[6], R_xt[s_]), (R["xn"],))
            if last and final_norm:
                rmsnorm_stats(xn[:, :], R["xn"])
                dve(lambda e: e.scalar_tensor_tensor(xt[s_][:, :], xn[:, :], RSTD, fnw[:, :], ALU.mult, ALU.mult), (R["xn"], R_sm["rstd"], R["consts"]), (R_xt[s_],))
                dma(lambda e: e.dma_start(out=out_d[i * 128:(i + 1) * 128, :], in_=xt[s_][:, :]), (R_xt[s_],), (), "ost%d" % s_)
            elif last:
                dma(lambda e: e.dma_start(out=out_d[i * 128:(i + 1) * 128, :], in_=xn[:, :]), (R["xn"],), (), "ost0")
            else:
                dma(lambda e: e.dma_start(out=xs_d[l % 2][i * 128:(i + 1) * 128, :], in_=xn[:, :]), (R["xn"],), (R_xs[l % 2][i],), "ost0")

        def idx_part(i):
            s_ = i % 2
            q0 = i * 128
            S = q0 + 128
            pks = pk[s_]
            Rpk = R_pk[s_]
            dma(lambda e: e.dma_start(out=pk[s_][:, :], in_=pack_d[i, :, 0:2048]), (R_packd[i],), (R_pk[s_],), "pk%d" % s_)
            dma(lambda e: e.dma_start(out=pkg[i % 3][:, :], in_=pack_d[i, :, 2048:3072]), (R_packd[i],), (R_pkg[i % 3],), "pkg%d" % (i % 3))
            if i < 2:
                return
            dve(lambda e: e.tensor_tensor(diagW[:, :, :], ident.unsqueeze(1).broadcast_to([128, 8, 128]),
                                          wi_all[:, i, :].unsqueeze(2).broadcast_to([128, 8, 128]), ALU.mult), (R["consts"],), (R["diagW"],))
            nch = (S + 511) // 512
            for c in range(nch):
                c0 = c * 512
                w = min(512, S - c0)
                pend = []
                for h in range(8):
                    sbk = (0, 1, 7, 6)[ibank[0] % 4]
                    ibank[0] += 1
                    ri = rti[0] % 4
                    rti[0] += 1
                    pe(lambda e: e.matmul(ps[sbk][:, 0:w], pks[:, PK_QI + h * 128:PK_QI + (h + 1) * 128], kiT[:, c0:c0 + w], start=True, stop=True),
                       (Rpk,), (R_ps[sbk],))
                    act(lambda e: e.activation(Rt[ri][:, 0:w], ps[sbk][:, 0:w], AF.Relu), (R_ps[sbk],), (R_Rt[ri],))
                    pend.append((h, ri))
                    if len(pend) > 3:
                        ph, pri = pend.pop(0)
                        pe(lambda e: e.matmul(ps[4][:, 0:w], diagW[:, ph, :], Rt[pri][:, 0:w], start=(ph == 0), stop=False), (R_Rt[pri], R["diagW"]), (R_ps[4],))
                for ph, pri in pend:
                    pe(lambda e: e.matmul(ps[4][:, 0:w], diagW[:, ph, :], Rt[pri][:, 0:w], start=(ph == 0), stop=(ph == 7)), (R_Rt[pri], R["diagW"]), (R_ps[4],))
                dve(lambda e: e.tensor_copy(score[:, c0:c0 + w], ps[4][:, 0:w]), (R_ps[4],), (R["score"],))

        def bis_part(i):
            s_ = i % 2
            q0 = i * 128
            S = q0 + 128
            maskA = maskA2[s_]
            RmA = R_mA[s_]
            if i < 2:
                if q0 > 0:
                    pool(lambda e: e.memset(maskA[:, 0:q0], 0.0), (), (RmA,))
                pool(lambda e: e.tensor_copy(maskA[:, q0:q0 + 128], causb), (R["consts"],), (RmA,))
                return
            dve(lambda e: e.tensor_tensor(score[:, q0:q0 + 128], score[:, q0:q0 + 128], causb, ALU.add), (R["score"], R["consts"]), (R["score"],))
            dve(lambda e: e.tensor_reduce(HI, score[:, 0:S], AX.X, ALU.max), (R["score"],), (R_sm["hi"],))
            dve(lambda e: e.tensor_reduce(LO, score[:, 0:256], AX.X, ALU.min), (R["score"],), (R_sm["lo"],))
            dve(lambda e: e.tensor_tensor(W0, HI, LO, ALU.subtract), (R_sm["hi"], R_sm["lo"]), (R_sm["w0"],))
            dve(lambda e: e.tensor_scalar(WK, cf[:, CF_POW:CF_POW + 16], W0, None, ALU.mult), (R_sm["w0"], R["consts"]), (R_sm["wk"],))
            dve(lambda e: e.tensor_tensor(TB[0], LO, WK[:, 0:1], ALU.add), (R_sm["lo"], R_sm["wk"]), (R_sm["t0"],))
            for k_ in range(NBIS):
                ta, tb_ = TB[k_ % 2], TB[(k_ + 1) % 2]
                Ra, Rb = R_sm["t%d" % (k_ % 2)], R_sm["t%d" % ((k_ + 1) % 2)]
                dve(lambda e: e.tensor_scalar(junk[:, 0:S], score[:, 0:S], ta, 0.0, ALU.is_ge, ALU.add, accum_out=CNT), (R["score"], Ra), (R["junk"], R_sm["cnt"]))
                dve(lambda e: e.tensor_scalar(UU, CNT, TOPK - 0.5, WK[:, k_:k_ + 1], ALU.is_ge, ALU.mult), (R_sm["cnt"], R_sm["wk"]), (R_sm["uu"],))
                dve(lambda e: e.scalar_tensor_tensor(tb_, UU, WK[:, k_ + 1:k_ + 2], ta, ALU.subtract, ALU.add), (R_sm["uu"], R_sm["wk"], Ra), (Rb,))
            tf = TB[NBIS % 2]
            Rf = R_sm["t%d" % (NBIS % 2)]
            dve(lambda e: e.tensor_scalar(maskA[:, 0:S], score[:, 0:S], tf, NEG, ALU.is_lt, ALU.mult), (R["score"], Rf), (RmA,))

        idx_part(0)
        bis_part(0)
        for i in range(nt_run):
            s_ = i % 2
            q0 = i * 128
            S = q0 + 128
            pks = pk[s_]
            Rpk = R_pk[s_]
            maskA, maskB = maskA2[s_], maskB2[s_]
            R["maskA"], R["maskB"] = R_mA[s_], R_mB[s_]
            dma(lambda e, s_=s_, i=i: e.dma_start(out=xt[s_][:, :], in_=xsrc[i * 128:(i + 1) * 128, :]),
                () if R_xsrc is None else (R_xsrc[i],), (R_xt[s_],), "xt%d" % s_)
            if i + 1 < nt_run:
                idx_part(i + 1)
            if i > 0:
                out_part(i - 1)
            if i > 0:
                pool(lambda e, i=i: e.memset(maskC[:, 8 * (i - 1):8 * (i - 1) + 8], 0.0), (), (R["maskC"],))
            pool(lambda e, i=i: e.tensor_copy(maskC[:, 8 * i:8 * i + 8], cb[:, CB_P8:CB_P8 + 8]), (R["consts"],), (R["maskC"],))
            ntc = 1 if 8 * i + 7 <= 128 else 2
            obu = 2 + obank[0] % 2
            obank[0] += 1
            for nt in range(ntc):
                sbk = sbank[0] % 2
                sbank[0] += 1
                nsl = slice(nt * 128, (nt + 1) * 128)
                pe(lambda e, sbk=sbk, nsl=nsl: e.matmul(ps[sbk][:, 0:256], kcmpT[:, nsl], pks[:, PK_QB:PK_QB + 256], start=True, stop=False, skip_group_check=True), (Rpk, R["kcmpT"]), (R_ps[sbk],))
                pe(lambda e, sbk=sbk, nsl=nsl: e.matmul(ps[sbk][:, 256:512], kcmpT[:, nsl], pks[:, PK_QB + 256:PK_QB + 512], start=False, stop=False, skip_group_check=True), (Rpk,), (R_ps[sbk],))
                pe(lambda e, sbk=sbk, nt=nt: e.matmul(ps[sbk][:, 0:512], maskC[:, 1 + nt * 128:1 + (nt + 1) * 128], I4, start=False, stop=True, skip_group_check=True), (R["maskC"], R["consts"]), (R_ps[sbk],))
                pi = pti[0] % 3
                pti[0] += 1
                act(lambda e, sbk=sbk, pi=pi: e.activation(pT[pi][:, :], ps[sbk][:, :], AF.Exp, scale=0.125), (R_ps[sbk],), (R_pT[pi],))
                for h in range(4):
                    pe(lambda e, h=h, pi=pi, nt=nt: e.matmul(ps[obu][:, h * 128:(h + 1) * 128], pT[pi][:, h * 128:(h + 1) * 128], vcmp[:, nt, :],
                                                              start=(nt == 0 and h == 0), stop=(nt == ntc - 1), skip_group_check=True), (R_pT[pi], R["vcmp"]), (R_ps[obu],))
            Uv = r3(ps[obu][:, :], 128)
            dve(lambda e, Uv=Uv: e.tensor_scalar(DENC.unsqueeze(2), Uv[:, :, 64:65], 1e-30, None, ALU.max), (R_ps[obu],), (R_sm["denc"],))
            dve(lambda e: e.reciprocal(RDC, DENC), (R_sm["denc"],), (R_sm["rdc"],))
            dve(lambda e, Uv=Uv: e.tensor_scalar(imp[:, 1:64], Uv[:, 0, 65:128], RDC[:, 0:1], None, ALU.mult), (R_ps[obu], R_sm["rdc"]), (R["imp"],))
            for h in range(1, 4):
                dve(lambda e, Uv=Uv, h=h: e.scalar_tensor_tensor(imp[:, 1:64], Uv[:, h, 65:128], RDC[:, h:h + 1], imp[:, 1:64], ALU.mult, ALU.add), (R_ps[obu], R_sm["rdc"], R["imp"]), (R["imp"],))
            ko = 62 - 2 * i
            dve(lambda e, ko=ko: e.tensor_tensor(imp2[:, :], imp[:, :], cf[:, CF_KEEP + ko:CF_KEEP + ko + 64], ALU.mult), (R["imp"], R["consts"]), (R["imp2"],))
            dve(lambda e, ko=ko: e.tensor_tensor(imp2[:, :], imp2[:, :], cf[:, CF_ADD + ko:CF_ADD + ko + 64], ALU.add), (R["imp2"],), (R["imp2"],))
            dve(lambda e: e.max(M8A, imp2[:, :]), (R["imp2"],), (R_sm["m8a"],))
            dve(lambda e: e.match_replace(imp3[:, :], M8A, imp2[:, :], -3.0e38), (R["imp2"], R_sm["m8a"]), (R["imp3"],))
            dve(lambda e: e.max(M8B, imp3[:, :]), (R["imp3"],), (R_sm["m8b"],))
            nb = 2 * (i + 1)
            dve(lambda e, nb=nb, S=S: e.tensor_scalar(r3(maskB[:, 0:S], 64), imp2[:, 0:nb].unsqueeze(2).broadcast_to([128, nb, 64]), M8B[:, 7:8], NEG, ALU.is_lt, ALU.mult),
                (R["imp2"], R_sm["m8b"]), (R["maskB"],))
            dve(lambda e, q0=q0: e.tensor_tensor(maskB[:, q0:q0 + 128], maskB[:, q0:q0 + 128], causb, ALU.add), (R["maskB"], R["consts"]), (R["maskB"],))
            gv = r3(g_all[:, i, :], 3)
            dve(lambda e, gv=gv: e.tensor_tensor(FC.unsqueeze(2), gv[:, :, 0:1], RDC.unsqueeze(2), ALU.mult), (R_sm["rdc"],), (R_sm["fc"],))
            dve(lambda e, Uv=Uv: e.tensor_tensor(r3(ob32[:, :], 64), Uv[:, :, 0:64], FC.unsqueeze(2).broadcast_to([128, 4, 64]), ALU.mult), (R_ps[obu], R_sm["fc"]), (R["ob32"],))
            oba = attn_loop(list(range(i + 1)), kaT, PK_QA, 0, lambda kt: (maskA[:, kt * 128:(kt + 1) * 128], R["maskA"]), pks, Rpk)
            acc = r3(ps[oba][:, 0:260], 65)
            dve(lambda e, acc=acc: e.reciprocal(RDA.unsqueeze(2), acc[:, :, 64:65]), (R_ps[oba],), (R_sm["rda"],))
            dve(lambda e, acc=acc: e.tensor_tensor(r3(oa_bf[:, :], 64), acc[:, :, 0:64], RDA.unsqueeze(2).broadcast_to([128, 4, 64]), ALU.mult), (R_ps[oba], R_sm["rda"]), (R["oa"],))
            if i + 1 < nt_run:
                bis_part(i + 1)
            obs = attn_loop(list(range(i + 1)), ksT, PK_QB, 1, lambda kt: (maskB[:, kt * 128:(kt + 1) * 128], R["maskB"]), pks, Rpk)
            acc = r3(ps[obs][:, 0:260], 65)
            dve(lambda e, acc=acc: e.reciprocal(RDS.unsqueeze(2), acc[:, :, 64:65]), (R_ps[obs],), (R_sm["rds"],))
            dve(lambda e, gv=gv: e.tensor_tensor(FS.unsqueeze(2), gv[:, :, 1:2], RDS.unsqueeze(2), ALU.mult), (R_sm["rds"],), (R_sm["fs"],))
            dve(lambda e, acc=acc: e.tensor_tensor(r3(tmp32[:, :], 64), acc[:, :, 0:64], FS.unsqueeze(2).broadcast_to([128, 4, 64]), ALU.mult), (R_ps[obs], R_sm["fs"]), (R["tmp32"],))
            dve(lambda e: e.tensor_tensor(ob32[:, :], ob32[:, :], tmp32[:, :], ALU.add), (R["ob32"], R["tmp32"]), (R["ob32"],))
            def wmask(kt, i=i):
                d_ = i - kt
                if d_ == 0:
                    return (causb, R["consts"])
                if d_ == 4:
                    return (win4b, R["consts"])
                return None
            obw = attn_loop(list(range(max(0, i - 4), i + 1)), kwT, PK_QB, 2, wmask, pks, Rpk)
            acc = r3(ps[obw][:, 0:260], 65)
            dve(lambda e, acc=acc: e.reciprocal(RDW.unsqueeze(2), acc[:, :, 64:65]), (R_ps[obw],), (R_sm["rdw"],))
            dve(lambda e, gv=gv: e.tensor_tensor(FW.unsqueeze(2), gv[:, :, 2:3], RDW.unsqueeze(2), ALU.mult), (R_sm["rdw"],), (R_sm["fw"],))
            dve(lambda e, acc=acc: e.tensor_tensor(r3(tmp32[:, :], 64), acc[:, :, 0:64], FW.unsqueeze(2).broadcast_to([128, 4, 64]), ALU.mult), (R_ps[obw], R_sm["fw"]), (R["tmp32"],))
            dve(lambda e: e.tensor_tensor(ob_bf[:, :], ob32[:, :], tmp32[:, :], ALU.add), (R["ob32"], R["tmp32"]), (R["ob"],))
        out_part(nt_run - 1)
        P.barrier()
    P.build(nc, es)
    es.close()
    return nc


_CACHE = {}


def _host_prep(norm_w, w_in, w_out, conv_w, pe_cmp, w_cmp_k, w_cmp_v, pool_w, pool_scale, final_norm_w):
    ci = _colidx()
    w_in_p = np.ascontiguousarray(np.asarray(w_in, np.float32)[:, :, ci])
    smallp = np.zeros((DEPTH, 128, SPW), np.float32)
    wcmp = np.zeros((DEPTH, 128, 32, 128), np.float32)
    for l in range(DEPTH):
        smallp[l, :, SP_NW:SP_NW + 8] = np.asarray(norm_w[l]).reshape(8, 128).T
        smallp[l, :, SP_CONV:SP_CONV + 6] = np.asarray(conv_w[l]).T.reshape(2, 128, 3).transpose(1, 0, 2).reshape(128, 6)
        smallp[l, :, SP_PE:SP_PE + 32] = np.tile(np.asarray(pe_cmp[l]).T, (2, 1))
        smallp[l, :, SP_PS:SP_PS + 2] = np.asarray(pool_scale[l]).reshape(2, 128).T
        for g in range(4):
            o = SP_POOLW + g * 128 + (g % 2) * 64
            smallp[l, 0:64, o:o + 64] = np.asarray(pool_w[l, g])
        wk = np.asarray(w_cmp_k[l]).reshape(32, 64, 64).transpose(1, 0, 2)
        wv = np.asarray(w_cmp_v[l]).reshape(32, 64, 64).transpose(1, 0, 2)
        wcmp[l, 0:64, :, 0:64] = wk
        wcmp[l, 0:64, :, 64:128] = wk
        wcmp[l, 64:128, :, 0:64] = wv
    return dict(w_in=w_in_p, w_out=np.ascontiguousarray(np.asarray(w_out, np.float32)), wcmp=wcmp.reshape(DEPTH, 128, 4096),
                smallp=smallp, fnw=np.asarray(final_norm_w, np.float32).reshape(1, D))


def run(inputs, n_layers=DEPTH, final_norm=True, cores=8, dbg_pack=False, trace=False, nt_run=NT, stop_after=None, p1_cut=None):
    key = (n_layers, final_norm, dbg_pack, nt_run, stop_after, p1_cut)
    if key not in _CACHE:
        _CACHE[key] = build_program(n_layers, final_norm, dbg_pack, nt_run, stop_after, p1_cut)
    nc = _CACHE[key]
    shared = _host_prep(*[inputs[k] for k in ["norm_w", "w_in", "w_out", "conv_w", "pe_cmp", "w_cmp_k", "w_cmp_v", "pool_w", "pool_scale", "final_norm_w"]])
    cfc, cbc = _consts()
    shared["cst_f"] = cfc
    shared["cst_b"] = cbc
    x = np.asarray(inputs["x"], np.float32)
    in_maps = []
    for c in range(cores):
        m = dict(shared)
        m["x"] = np.ascontiguousarray(x[c])
        in_maps.append(m)
    kw = {"trace": True} if trace else {}
    res = run_bass_kernel_spmd(nc, in_maps, core_ids=list(range(cores)), **kw)
    return res


def kernel(x, norm_w, w_in, w_out, conv_w, pe_cmp, w_cmp_k, w_cmp_v, pool_w, pool_scale, final_norm_w):
    inputs = dict(x=x, norm_w=norm_w, w_in=w_in, w_out=w_out, conv_w=conv_w, pe_cmp=pe_cmp, w_cmp_k=w_cmp_k,
                  w_cmp_v=w_cmp_v, pool_w=pool_w, pool_scale=pool_scale, final_norm_w=final_norm_w)
    res = run(inputs)
    return np.stack([np.asarray(r["out"], np.float32) for r in res.results], axis=0)
```

```python
import contextlib
import numpy as np
import ml_dtypes
import concourse.bass as bass
import concourse.mybir as mybir
from concourse.bass_utils import run_bass_kernel_spmd

F32 = mybir.dt.float32
BF16 = mybir.dt.bfloat16
F16 = mybir.dt.float16
ALU = mybir.AluOpType
AF = mybir.ActivationFunctionType
AX = mybir.AxisListType

T = 4096
D = 1024
NT = 32
DEPTH = 4
NEG = -30000.0
BIG = 1.0e30
TOPK = 256
NBIS = 9

O_R16, O_IDX, O_KCVC, O_DU, O_V, O_WI, O_G = 0, 896, 1280, 1408, 1664, 1856, 1864
TMW = 1876
O_FM = TMW
WCOLS = TMW + 14 * 128
PIECES = [(0, 1024), (1024, 1024), (2048, 1024), (3072, WCOLS - 3072)]
PK_QA, PK_QB, PK_QI, PK_G = 0, 512, 1024, 2048
PKW = 3072
SP_NW, SP_CONV, SP_PE, SP_PS, SP_POOLW, SPW = 0, 8, 14, 46, 48, 560
CF_COS16, CF_SIN16, CF_COS8, CF_SIN8 = 0, 256, 512, 640
CF_CC, CF_SC, CF_PROT, CF_KEEP, CF_ADD, CF_RM, CF_CAUS = 768, 1024, 1280, 1408, 1536, 1664, 1672
CF_POW = 1800
CFW = 1816
CB_ID, CB_I4, CB_CAUS, CB_WIN4, CB_P8, CB_BAND, CB_OVL = 0, 128, 640, 768, 896, 904, 2440
CB_MQ, CB_MI = 2568, 2824
CBW = 3336


def _in_offsets():
    names = [("a_q", 256), ("a_k", 64), ("a_v", 64), ("a_qi", 256), ("a_ki", 32), ("a_wi", 8), ("a_gate", 256),
             ("b_q", 256), ("b_kc", 64), ("b_vc", 64), ("b_ks", 64), ("b_vs", 64), ("b_kw", 64), ("b_vw", 64),
             ("b_g", 12), ("b_gate", 256), ("c_b", 256), ("c_c", 256), ("c_x", 256), ("c_gate", 256),
             ("d_u", 256), ("d_gate", 256)]
    o = {}
    c = 0
    for n, w in names:
        o[n] = (c, w)
        c += w
    return o


def _colidx():
    o = _in_offsets()
    r = lambda n, a=0, b=None: list(range(o[n][0] + a, o[n][0] + (o[n][1] if b is None else b)))
    idx = []
    idx += r("a_q")
    idx += r("a_k") + r("a_k")
    idx += r("b_q")
    idx += r("b_ks") + r("b_ks")
    idx += r("b_kw") + r("b_kw")
    idx += r("a_qi")
    idx += r("a_ki") * 4
    idx += r("b_kc") + r("b_vc")
    idx += r("d_u")
    idx += r("a_v") + r("b_vs") + r("b_vw")
    idx += r("a_wi") + r("b_g")
    assert len(idx) == TMW
    for n in ["a_gate", "b_gate", "c_b", "c_c", "c_x", "c_gate", "d_gate"]:
        idx += r(n)
    assert len(idx) == WCOLS
    return np.array(idx, dtype=np.int64)


def _consts():
    cf = np.zeros((128, CFW), np.float32)
    pos = np.arange(T, dtype=np.float32)
    for (half, oc, os_) in [(8, CF_COS16, CF_SIN16), (4, CF_COS8, CF_SIN8)]:
        inv = (np.float32(500000.0) ** (-(np.arange(half, dtype=np.float32) / np.float32(half)))).astype(np.float32)
        ang = (pos[:, None] * inv[None, :]).astype(np.float32)
        c = np.cos(ang.astype(np.float64)).astype(np.float32).reshape(NT, 128, half).transpose(1, 0, 2).reshape(128, NT * half)
        s = np.sin(ang.astype(np.float64)).astype(np.float32).reshape(NT, 128, half).transpose(1, 0, 2).reshape(128, NT * half)
        cf[:, oc:oc + NT * half] = c
        cf[:, os_:os_ + NT * half] = s
    inv = (np.float32(500000.0) ** (-(np.arange(8, dtype=np.float32) / np.float32(8)))).astype(np.float32)
    cpos = (np.arange(255) * 16 + 31).astype(np.float32)
    ang = (cpos[:, None] * inv[None, :]).astype(np.float32).astype(np.float64)
    cc = np.ones((64, 256), np.float32)
    sc = np.zeros((64, 256), np.float32)
    for e in range(16):
        cc[e, :255] = np.cos(ang[:, e % 8])
        sc[e, :255] = np.sin(ang[:, e % 8])
    cf[:, CF_CC:CF_CC + 256] = np.tile(cc, (2, 1))
    cf[:, CF_SC:CF_SC + 256] = np.tile(sc, (2, 1))
    prot = np.zeros((64, 64), np.float32)
    for e in range(8):
        prot[e + 8, e] = -1.0
        prot[e, e + 8] = 1.0
    p128 = np.zeros((128, 128), np.float32)
    p128[:64, :64] = prot
    p128[64:, 64:] = prot
    cf[:, CF_PROT:CF_PROT + 128] = p128
    keep = np.zeros((128, 128), np.float32)
    add = np.zeros((128, 128), np.float32)
    for q in range(128):
        cur = 0 if q < 64 else 1
        for col in range(126):
            r = col - 62
            if r < cur:
                keep[q, col] = 1.0
            elif r == cur:
                add[q, col] = BIG
            else:
                add[q, col] = -BIG
    cf[:, CF_KEEP:CF_KEEP + 128] = keep
    cf[:, CF_ADD:CF_ADD + 128] = add
    p = np.arange(128)
    cf[:, CF_RM + 0] = (p < 64)
    cf[:, CF_RM + 1] = (p >= 64)
    for h in range(4):
        cf[:, CF_RM + 2 + h] = (p // 32 == h)
    qq, ss = np.meshgrid(np.arange(128), np.arange(128), indexing="ij")
    cf[:, CF_CAUS:CF_CAUS + 128] = np.where(ss <= qq, 0.0, -BIG)
    for k in range(16):
        cf[:, CF_POW + k] = 2.0 ** (-(k + 1))

    cb = np.zeros((128, CBW), np.float32)
    cb[:, CB_ID:CB_ID + 128] = np.eye(128)
    cb[:, CB_I4:CB_I4 + 512] = np.tile(np.eye(128), (1, 4))
    cb[:, CB_CAUS:CB_CAUS + 128] = np.where(ss <= qq, 0.0, NEG)
    cb[:, CB_WIN4:CB_WIN4 + 128] = np.where(ss > qq, 0.0, NEG)
    for c in range(8):
        m = c - 1
        cb[:, CB_P8 + c] = np.where(np.arange(128) >= 16 * m + 31, 0.0, NEG)
    sl, tl = np.meshgrid(np.arange(128), np.arange(128), indexing="ij")
    for g, w in enumerate((2, 4, 8, 16)):
        cur = ((sl >= tl - w + 1) & (sl <= tl)) / float(w) - (sl == tl)
        prev = ((sl - 128) >= (tl - w + 1)) / float(w)
        cnt0 = np.minimum(tl + 1, w).astype(np.float64)
        cur0 = ((sl >= tl - w + 1) & (sl <= tl)) / cnt0 - (sl == tl)
        for k, m in enumerate((cur, prev, cur0)):
            o = CB_BAND + (g * 3 + k) * 128
            cb[:, o:o + 128] = m
    n = np.arange(256)
    for j in range(1, 64):
        ov = ((16 * n < 64 * j + 64) & (16 * n + 31 >= 64 * j) & (n < 255)).astype(np.float32)
        cb[:, CB_OVL + (j - 1)] = ov[:128]
        cb[:, CB_OVL + 64 + (j - 1)] = ov[128:]
    cb[:, CB_MQ:CB_MQ + 128] = (p < 64)[:, None]
    cb[:, CB_MQ + 128:CB_MQ + 256] = (p >= 64)[:, None]
    for h in range(4):
        cb[:, CB_MI + h * 128:CB_MI + (h + 1) * 128] = (p // 32 == h)[:, None]
    return cf, cb.astype(ml_dtypes.bfloat16)


class Region:
    __slots__ = ("name", "w", "r")

    def __init__(self, name):
        self.name = name
        self.w = None
        self.r = {}


ENGS = ("pe", "act", "dve", "pool", "sp")


class _Rec:
    def __init__(self):
        self.call = None

    def __getattr__(self, name):
        def f(*a, **k):
            self.call = (name, a, k)
            return None
        return f


class Prog:
    def __init__(self):
        self.ops = {e: [] for e in ENGS}
        self.cnt = {e: 0 for e in ENGS}
        self.known = {e: {} for e in ENGS}
        self.dma_cnt = {}
        self.waited = {e: set() for e in ENGS}

    def emit(self, eng, fn, r=(), w=(), slot=None):
        d = {}

        def add(ev, raw):
            if ev is None:
                return
            k, v = ev
            if k == eng and eng == "pe":
                return
            if d.get(k, 0) < v:
                d[k] = v
        for R in r:
            add(R.w, True)
        for R in w:
            add(R.w, False)
            for k, v in R.r.items():
                add((k, v), False)
        kn = self.known[eng]
        for k, v in d.items():
            if kn.get(k, 0) < v:
                kn[k] = v
                self.ops[eng].append(("wait", k, v))
                if k in self.waited:
                    self.waited[k].add(v)
        rec = _Rec()
        fn(rec)
        fn = rec.call
        assert fn is not None
        if slot is None:
            self.cnt[eng] += 1
            ev = (eng, self.cnt[eng])
            self.ops[eng].append(("op", fn, self.cnt[eng]))
        else:
            c = self.dma_cnt.get(slot, 0) + 16
            self.dma_cnt[slot] = c
            ev = (slot, c)
            self.ops[eng].append(("dma", fn, slot))
        for R in r:
            if R.r.get(ev[0], 0) < ev[1]:
                R.r[ev[0]] = ev[1]
        for R in w:
            R.w = ev
            R.r = {}
        return ev

    def barrier(self):
        targets = {e: self.cnt[e] for e in ENGS if self.cnt[e] > 0}
        for s, c in self.dma_cnt.items():
            targets[s] = c
        for eng in ENGS:
            kn = self.known[eng]
            for k, v in targets.items():
                if k == eng:
                    continue
                if kn.get(k, 0) < v:
                    kn[k] = v
                    self.ops[eng].append(("wait", k, v))
                    if k in self.waited:
                        self.waited[k].add(v)

    def build(self, nc, es):
        sems = {e: es.enter_context(nc.semaphore("sem_" + e)) for e in ENGS}
        for s in self.dma_cnt:
            sems[s] = es.enter_context(nc.semaphore("dsem_" + s))
        rank = {}
        for e in ENGS:
            rank[e] = {v: i + 1 for i, v in enumerate(sorted(self.waited[e]))}

        def replay(handle, e):
            for op in self.ops[e]:
                if op[0] == "wait":
                    k, v = op[1], op[2]
                    val = rank[k][v] if k in rank else v
                    handle.wait_ge(sems[k], val)
                elif op[0] == "op":
                    name, a, k = op[1]
                    ins = getattr(handle, name)(*a, **k)
                    if op[2] in rank[e]:
                        ins.then_inc(sems[e], 1)
                else:
                    name, a, k = op[1]
                    getattr(handle, name)(*a, **k).then_inc(sems[op[2]], 16)

        with nc.Block() as block:
            @block.tensor
            def _(h):
                replay(h, "pe")

            @block.scalar
            def _(h):
                replay(h, "act")

            @block.vector
            def _(h):
                replay(h, "dve")

            @block.gpsimd
            def _(h):
                replay(h, "pool")

            @block.sync
            def _(h):
                replay(h, "sp")


def r3(ap, b):
    return ap.rearrange("p (a b) -> p a b", b=b)


def build_program(n_layers=DEPTH, final_norm=True, dbg_pack=False, nt_run=NT, stop_after=None, p1_cut=None):
    nc = bass.Bass("TRN2", target_bir_lowering=False)
    P = Prog()
    es = contextlib.ExitStack()
    dram = lambda n, s, dt, kind: nc.dram_tensor(n, s, dt, kind=kind).ap()
    x_in = dram("x", [T, D], F32, "ExternalInput")
    w_in_d = dram("w_in", [DEPTH, D, WCOLS], F32, "ExternalInput")
    w_out_d = dram("w_out", [DEPTH, D, D], F32, "ExternalInput")
    wcmp_d = dram("wcmp", [DEPTH, 128, 4096], F32, "ExternalInput")
    smallp_d = dram("smallp", [DEPTH, 128, SPW], F32, "ExternalInput")
    fnw_d = dram("fnw", [1, D], F32, "ExternalInput")
    cf_d = dram("cst_f", [128, CFW], F32, "ExternalInput")
    cb_d = dram("cst_b", [128, CBW], BF16, "ExternalInput")
    out_d = dram("out", [T, D], F32, "ExternalOutput")
    pack_d = dram("pack_d", [NT, 128, PKW], BF16, "ExternalOutput" if dbg_pack else "Internal")
    xs_d = [dram("xs0", [T, D], F32, "Internal"), dram("xs1", [T, D], F32, "Internal")]
    R_packd = [Region("packd%d" % i) for i in range(NT)]
    R_xs = [[Region("xs%d_%d" % (k, i)) for i in range(NT)] for k in range(2)]

    def sb(name, shape, dt):
        return es.enter_context(nc.sbuf_tensor("s_" + name, shape, dt))

    arena1 = sb("arena1", [128, 8 * WCOLS], BF16)
    arena2 = sb("arena2", [128, 7936], F32)
    w_out = sb("w_out", [128, 8, D], BF16)
    kaT = sb("kaT", [128, T], BF16)
    ksT = sb("ksT", [128, T], BF16)
    kwT = sb("kwT", [128, T], BF16)
    kiT = sb("kiT", [128, T], BF16)
    V_all = sb("V_all", [128, NT, 3, 65], BF16)
    xt = [sb("xt%d" % i, [128, D], F32) for i in range(2)]
    hb2 = [sb("hb%d" % i, [128, D], BF16) for i in range(2)]
    hT2 = [sb("hT%d" % i, [128, 8, 128], BF16) for i in range(2)]
    hb = hb2[0]
    cf = sb("cf", [128, CFW], F32)
    cb = sb("cb", [128, CBW], BF16)
    smallp = sb("smallp", [128, SPW], F32)
    poolw = sb("poolw", [128, 512], BF16)
    fnw = sb("fnw", [128, D], F32)
    wi_all = sb("wi_all", [128, NT, 8], F32)
    g_all = sb("g_all", [128, NT, 12], F32)
    kcmpT = sb("kcmpT", [128, 256], BF16)
    vcmp = sb("vcmp", [128, 2, 128], BF16)
    maskC = sb("maskC", [128, 264], BF16)
    du = [sb("du%d" % i, [128, 256], BF16) for i in range(2)]
    uext = sb("uext", [128, 2, 130], F32)
    ccs = sb("ccs", [128, 256], F32)
    sgc = sb("sgc", [128, 256], F32)
    ysb = sb("ysb", [128, 2, 128], F32)
    tcb = sb("tcb", [128, 256], F32)
    sgd = sb("sgd", [128, 256], F32)
    pooled = sb("pooled", [128, 512], BF16)
    rt = sb("rt", [128, 4, 112], F32)
    rt2 = sb("rt2", [128, 4, 48], F32)
    sm = sb("sm", [128, 128], F32)
    imp = sb("imp", [128, 64], F32)
    imp2 = sb("imp2", [128, 64], F32)
    imp3 = sb("imp3", [128, 64], F32)
    ps = [es.enter_context(nc.psum_tensor("ps%d" % i, [128, 512], F32)) for i in range(8)]
    psb = [p_[:, :].bitcast(BF16) for p_ in ps]
    R_ps = [Region("ps%d" % i) for i in range(8)]

    w_in = r3(arena1[:, :], WCOLS)
    score = arena1[:, 0:4096].bitcast(F16)
    maskA2 = [arena1[:, 8192:12288], arena1[:, 20480:24576]]
    maskB2 = [arena1[:, 12288:16384], arena1[:, 24576:28672]]
    junk = arena1[:, 16384:20480]
    wcmp = r3(arena1[:, 16384:20480], 128)
    kcvc_tmp = [arena1[:, 20480 + 256 * i: 20480 + 256 * (i + 1)] for i in range(3)]
    kc_sb = arena1[:, 21504:22016].bitcast(F32)
    kc_t1 = arena1[:, 22016:22528].bitcast(F32)
    kc_t2 = arena1[:, 22528:23040].bitcast(F32)
    a2 = arena2
    wst = [a2[:, 1024 * i:1024 * (i + 1)] for i in range(4)]
    zs = a2[:, 0:TMW]
    zb = a2[:, 1880:1880 + 704].bitcast(BF16)
    packt = [a2[:, 2600 + 1536 * i: 2600 + 1536 * (i + 1)].bitcast(BF16) for i in range(2)]
    kcvcT = a2[:, 5680:5680 + 2048].bitcast(BF16)
    pk = [a2[:, 1024 * i:1024 * (i + 1)].bitcast(BF16) for i in range(2)]
    pkg = [a2[:, 2048 + 512 * i:2048 + 512 * (i + 1)].bitcast(BF16) for i in range(3)]
    Rt = [a2[:, 3584 + 256 * k:3584 + 256 * (k + 1)].bitcast(BF16) for k in range(3)] + [a2[:, 7680:7936].bitcast(BF16)]
    pT = [a2[:, 4352 + 256 * k:4352 + 256 * (k + 1)].bitcast(BF16) for k in range(3)]
    diagW = r3(a2[:, 5120:5632].bitcast(BF16), 128)
    oa_bf = a2[:, 5632:5760].bitcast(BF16)
    ob_bf = a2[:, 5760:5888].bitcast(BF16)
    ob32 = a2[:, 5888:6144]
    mx = a2[:, 6144:6400].bitcast(BF16)
    xn = a2[:, 6400:7424]
    tmp32 = a2[:, 7424:7680]

    ident = cb[:, CB_ID:CB_ID + 128]
    I4 = cb[:, CB_I4:CB_I4 + 512]
    causb = cb[:, CB_CAUS:CB_CAUS + 128]
    win4b = cb[:, CB_WIN4:CB_WIN4 + 128]

    SS, MS, SD, RSTD = sm[:, 0:1], sm[:, 1:2], sm[:, 2:3], sm[:, 3:4]
    HI, LO, W0 = sm[:, 4:5], sm[:, 5:6], sm[:, 6:7]
    WK = sm[:, 8:24]
    TB = [sm[:, 24:25], sm[:, 25:26]]
    CNT, UU = sm[:, 26:27], sm[:, 27:28]
    RDA, DENC, RDC, FC = sm[:, 32:36], sm[:, 36:40], sm[:, 40:44], sm[:, 44:48]
    RDS, FS, RDW, FW = sm[:, 48:52], sm[:, 52:56], sm[:, 56:60], sm[:, 60:64]
    M8A, M8B = sm[:, 64:72], sm[:, 72:80]
    R_sm = {k: Region("sm_" + k) for k in ["ss", "ms", "sd", "rstd", "hi", "lo", "w0", "wk", "t0", "t1", "cnt", "uu",
                                            "rda", "denc", "rdc", "fc", "rds", "fs", "rdw", "fw", "m8a", "m8b"]}

    R = {k: Region(k) for k in ["hb", "hT", "zs", "zb", "rt", "uext", "ccs", "sgc", "ysb", "tcb", "sgd", "pooled",
                                "smallp", "poolw", "score", "maskA", "maskB", "maskC", "diagW", "oa", "ob", "ob32",
                                "mx", "xn", "tmp32", "imp", "imp2", "imp3", "kcmpT", "vcmp", "kc_sb", "kc_t1", "kc_t2",
                                "wcmp", "consts"]}
    R_xt = [Region("xt0"), Region("xt1")]
    R["rt2"] = Region("rt2")
    R["zbA"] = Region("zbA")
    R["zbB"] = Region("zbB")
    R_hb = [Region("hb0"), Region("hb1")]
    R_hT = [Region("hT0"), Region("hT1")]
    R_du = [Region("du0"), Region("du1")]
    R_wst = [Region("wst%d" % i) for i in range(4)]
    R_pkt = [{k: Region("pkt%d_%s" % (i, k)) for k in ["qa", "qb", "qi", "gab", "mc", "md"]} for i in range(2)]
    R_pk = [Region("pk0"), Region("pk1")]
    R_pkg = [Region("pkg%d" % i) for i in range(3)]
    R_Rt = [Region("Rt%d" % i) for i in range(4)]
    R_pT = [Region("pT%d" % i) for i in range(3)]
    R_tmpj = [Region("tmpj%d" % i) for i in range(3)]
    R_mA = [Region("maskA0"), Region("maskA1")]
    R_mB = [Region("maskB0"), Region("maskB1")]
    R["junk"] = Region("junk")

    act, dve, pool, pe = (lambda f, r=(), w=(): P.emit("act", f, r, w)), (lambda f, r=(), w=(): P.emit("dve", f, r, w)), \
        (lambda f, r=(), w=(): P.emit("pool", f, r, w)), (lambda f, r=(), w=(): P.emit("pe", f, r, w))

    def dma(f, r, w, slot, q="sp"):
        P.emit(q, f, r, w, slot=slot)

    dma(lambda e: e.dma_start(out=cf[:, :], in_=cf_d[:, :]), (), (R["consts"],), "c0")
    dma(lambda e: e.dma_start(out=cb[:, :], in_=cb_d[:, :]), (), (R["consts"],), "c1")
    dma(lambda e: e.dma_start(out=fnw[:, :], in_=fnw_d[0:1, :].broadcast_to([128, D])), (), (R["consts"],), "c2")
    pool(lambda e: e.memset(V_all[:, :, :, 64:65], 1.0))
    pool(lambda e: e.memset(kcmpT[:, :], 0.0), (), (R["kcmpT"],))
    pool(lambda e: e.memset(vcmp[:, :, :], 0.0), (), (R["vcmp"],))
    pool(lambda e: e.memset(vcmp[:, :, 64:65], 1.0), (), (R["vcmp"],))
    pool(lambda e: e.tensor_copy(vcmp[:, :, 65:128], r3(cb[:, CB_OVL:CB_OVL + 128], 64)[:, :, 0:63]), (R["consts"],), (R["vcmp"],))
    pool(lambda e: e.memset(imp[:, :], BIG), (), (R["imp"],))
    P.barrier()

    cast_rr = [0]

    def cast(out_ap, in_ap, scale_ap, r, w):
        k = cast_rr[0] % 3
        cast_rr[0] += 1
        if k == 0:
            if scale_ap is None:
                pool(lambda e: e.tensor_copy(out_ap, in_ap), r, w)
            else:
                pool(lambda e: e.tensor_scalar(out_ap, in_ap, scale_ap, None, ALU.mult), r, w)
        elif k == 1:
            if scale_ap is None:
                dve(lambda e: e.tensor_copy(out_ap, in_ap), r, w)
            else:
                dve(lambda e: e.tensor_scalar(out_ap, in_ap, scale_ap, None, ALU.mult), r, w)
        else:
            if scale_ap is None:
                act(lambda e: e.copy(out_ap, in_ap), r, w)
            else:
                act(lambda e: e.activation(out_ap, in_ap, AF.Identity, scale=scale_ap), r, w)

    sbank = [0]
    ibank = [0]
    obank = [0]
    rti = [0]
    pti = [0]

    def rmsnorm_stats(src_ap, r_src):
        act(lambda e: e.activation(hb[:, :], src_ap, AF.Square, accum_out=SS), (r_src,), (R["hb"], R_sm["ss"]))
        dve(lambda e: e.tensor_scalar(MS, SS, 1.0 / D, 1e-6, ALU.mult, ALU.add), (R_sm["ss"],), (R_sm["ms"],))
        act(lambda e: e.activation(SD, MS, AF.Sqrt), (R_sm["ms"],), (R_sm["sd"],))
        dve(lambda e: e.reciprocal(RSTD, SD), (R_sm["sd"],), (R_sm["rstd"],))

    for l in range(n_layers):
        xsrc = x_in if l == 0 else xs_d[(l - 1) % 2]
        R_xsrc = None if l == 0 else R_xs[(l - 1) % 2]
        last = (l == n_layers - 1)
        dma(lambda e: e.dma_start(out=smallp[:, :], in_=smallp_d[l, :, :]), (), (R["smallp"],), "smallp")
        pool(lambda e: e.tensor_copy(poolw[:, :], smallp[:, SP_POOLW:SP_POOLW + 512]), (R["smallp"],), (R["poolw"],))
        k = 0
        for kc in range(8):
            for (c0, w) in PIECES:
                s_ = k % 4
                k += 1
                dma(lambda e, s_=s_, kc=kc, c0=c0, w=w: e.dma_start(out=wst[s_][:, 0:w], in_=w_in_d[l, kc * 128:(kc + 1) * 128, c0:c0 + w]),
                    (), (R_wst[s_],), "wst%d" % s_, q=("sp" if k % 2 else "act"))
                cast(w_in[:, kc, c0:c0 + w], wst[s_][:, 0:w], smallp[:, SP_NW + kc:SP_NW + kc + 1], (R_wst[s_], R["smallp"]), ())
        for kc in range(8):
            s_ = k % 4
            k += 1
            dma(lambda e, s_=s_, kc=kc: e.dma_start(out=wst[s_][:, :], in_=w_out_d[l, kc * 128:(kc + 1) * 128, :]), (), (R_wst[s_],), "wst%d" % s_)
            cast(w_out[:, kc, :], wst[s_][:, :], None, (R_wst[s_],), ())
        pool(lambda e: e.memset(uext[:, :, 0:2], 0.0), (), (R["uext"],))
        pool(lambda e: e.memset(maskC[:, :], NEG), (), (R["maskC"],))
        P.barrier()
        if stop_after == "p0":
            break
        def norm_part(tt):
            s_ = tt % 2
            dma(lambda e: e.dma_start(out=xt[s_][:, :], in_=xsrc[tt * 128:(tt + 1) * 128, :]),
                () if R_xsrc is None else (R_xsrc[tt],), (R_xt[s_],), "xt%d" % s_)
            act(lambda e: e.activation(hb2[s_][:, :], xt[s_][:, :], AF.Square, accum_out=SS), (R_xt[s_],), (R_hb[s_], R_sm["ss"]))
            dve(lambda e: e.tensor_scalar(MS, SS, 1.0 / D, 1e-6, ALU.mult, ALU.add), (R_sm["ss"],), (R_sm["ms"],))
            act(lambda e: e.activation(SD, MS, AF.Sqrt), (R_sm["ms"],), (R_sm["sd"],))
            dve(lambda e: e.reciprocal(RSTD, SD), (R_sm["sd"],), (R_sm["rstd"],))
            act(lambda e: e.activation(hb2[s_][:, :], xt[s_][:, :], AF.Identity, scale=RSTD), (R_xt[s_], R_sm["rstd"]), (R_hb[s_],))
            for kc in range(8):
                pe(lambda e, kc=kc: e.transpose(psb[0][:, kc * 128:(kc + 1) * 128], hb2[s_][:, kc * 128:(kc + 1) * 128], ident),
                   (R_hb[s_], R["consts"]), (R_ps[0],))
            dve(lambda e: e.tensor_copy(hT2[s_][:, :, :], r3(psb[0][:, 0:1024], 128)), (R_ps[0],), (R_hT[s_],))

        norm_part(0)
        for tt in range(nt_run):
            s_ = tt % 2
            pkt = packt[s_]
            Rk = R_pkt[s_]
            hT = hT2[s_]
            R["hT"] = R_hT[s_]
            for ci, (c0, w) in enumerate([(0, 512), (512, 512), (1024, 512), (1536, TMW - 1536)]):
                bk = 1 + ci % 2
                for kc in range(8):
                    pe(lambda e, bk=bk, kc=kc, c0=c0, w=w: e.matmul(ps[bk][:, 0:w], hT[:, kc, :], w_in[:, kc, c0:c0 + w], start=(kc == 0), stop=(kc == 7)),
                       (R["hT"],), (R_ps[bk],))
                act(lambda e, bk=bk, c0=c0, w=w: e.copy(zs[:, c0:c0 + w], ps[bk][:, 0:w]), (R_ps[bk],), (R["zs"],))

            if tt + 1 < nt_run:
                norm_part(tt + 1)
            def fm_group(bk, blocks):
                for bi, blk in enumerate(blocks):
                    for kc in range(8):
                        pe(lambda e, bi=bi, blk=blk, kc=kc: e.matmul(ps[bk][:, bi * 128:(bi + 1) * 128], w_in[:, kc, O_FM + blk * 128:O_FM + (blk + 1) * 128],
                                                                      hT[:, kc, :], start=(bi == 0 and kc == 0), stop=(kc == 7), skip_group_check=True),
                           (R["hT"],), (R_ps[bk],))
            fm_group(3, [0, 1, 2, 3])
            act(lambda e, pkt=pkt: e.activation(pkt[:, PK_G:PK_G + 512], ps[3][:, 0:512], AF.Silu), (R_ps[3],), (Rk["gab"],))
            fm_group(4, [6, 7, 8, 9])
            act(lambda e: e.copy(ccs[:, :], ps[4][:, 0:256]), (R_ps[4],), (R["ccs"],))
            if tt > 0:
                dve(lambda e: e.tensor_copy(uext[:, :, 0:2], uext[:, :, 128:130]), (R["uext"],), (R["uext"],))
            dve(lambda e: e.tensor_tensor(uext[:, :, 2:130], r3(ps[4][:, 256:512], 128), r3(ccs[:, :], 128), ALU.mult),
                (R_ps[4], R["ccs"]), (R["uext"],))
            fm_group(3, [4, 5, 10, 11])
            act(lambda e: e.activation(sgc[:, :], ps[3][:, 256:512], AF.Silu), (R_ps[3],), (R["sgc"],))
            for j in range(2):
                cw = lambda q, j=j: smallp[:, SP_CONV + j * 3 + q:SP_CONV + j * 3 + q + 1]
                dve(lambda e, j=j, cw=cw: e.tensor_scalar(ysb[:, j, :], uext[:, j, 0:128], cw(0), None, ALU.mult), (R["uext"], R["smallp"]), (R["ysb"],))
                dve(lambda e, j=j, cw=cw: e.scalar_tensor_tensor(ysb[:, j, :], uext[:, j, 1:129], cw(1), ysb[:, j, :], ALU.mult, ALU.add), (R["uext"], R["ysb"]), (R["ysb"],))
                dve(lambda e, j=j, cw=cw: e.scalar_tensor_tensor(ysb[:, j, :], uext[:, j, 2:130], cw(2), ysb[:, j, :], ALU.mult, ALU.add), (R["uext"], R["ysb"]), (R["ysb"],))
            dve(lambda e: e.tensor_tensor(tcb[:, :], ps[3][:, 0:256], ysb[:, :, :].rearrange("p a b -> p (a b)"), ALU.mult), (R_ps[3], R["ysb"]), (R["tcb"],))
            pool(lambda e, pkt=pkt: e.tensor_tensor(pkt[:, PK_G + 512:PK_G + 768], tcb[:, :], sgc[:, :], ALU.mult), (R["tcb"], R["sgc"]), (Rk["mc"],))
            cs16 = lambda o, tt=tt: cf[:, o + tt * 8:o + tt * 8 + 8].unsqueeze(1).broadcast_to([128, 14, 8])
            cs8 = lambda o, tt=tt: cf[:, o + tt * 4:o + tt * 4 + 4].unsqueeze(1).broadcast_to([128, 12, 4])
            dve(lambda e, s_=s_: e.tensor_copy(du[s_][:, :], zs[:, O_DU:O_DU + 256]), (R["zs"],), (R_du[s_],))
            dve(lambda e, tt=tt: e.tensor_copy(V_all[:, tt, :, 0:64], r3(zs[:, O_V:O_V + 192], 64)), (R["zs"],), ())
            dve(lambda e, tt=tt: e.tensor_copy(wi_all[:, tt, :], zs[:, O_WI:O_WI + 8]), (R["zs"],), ())
            for (zin, zout, nh, hd, half, cs, oc, os_, eng, rtb, rtk, zk) in [
                (zs[:, 0:896], zb[:, 0:896], 14, 64, 8, cs16, CF_COS16, CF_SIN16, pool, rt, "rt", "zbA"),
                (zs[:, 896:1280], zb[:, 896:1280], 12, 32, 4, cs8, CF_COS8, CF_SIN8, dve, rt2, "rt2", "zbB")]:
                zi = r3(zin, hd)
                zo = r3(zout, hd)
                x1, x2 = zi[:, :, 0:half], zi[:, :, half:2 * half]
                n = nh * half
                t_ = [rtb[:, q, 0:n].rearrange("p (a b) -> p a b", b=half) for q in range(4)]
                rr, ww = (R["zs"], R["consts"]), (R[rtk],)
                eng(lambda e: e.tensor_tensor(t_[0], x1, cs(oc), ALU.mult), rr, ww)
                eng(lambda e: e.tensor_tensor(t_[1], x2, cs(os_), ALU.mult), rr, ww)
                eng(lambda e: e.tensor_tensor(t_[2], x2, cs(oc), ALU.mult), rr, ww)
                eng(lambda e: e.tensor_tensor(t_[3], x1, cs(os_), ALU.mult), rr, ww)
                eng(lambda e: e.tensor_tensor(zo[:, :, 0:half], t_[0], t_[1], ALU.subtract), (R[rtk],), (R[zk],))
                eng(lambda e: e.tensor_tensor(zo[:, :, half:2 * half], t_[2], t_[3], ALU.add), (R[rtk],), (R[zk],))
                eng(lambda e: e.tensor_copy(zo[:, :, 2 * half:hd], zi[:, :, 2 * half:hd]), (R["zs"],), (R[zk],))
            dve(lambda e: e.tensor_copy(zb[:, 1280:1408], zs[:, 1280:1408]), (R["zs"],), (R["zbB"],))
            act(lambda e, tt=tt: e.activation(g_all[:, tt, :], zs[:, O_G:O_G + 12], AF.Sigmoid), (R["zs"],), ())
            fm_group(4, [12, 13])
            act(lambda e: e.activation(sgd[:, :], ps[4][:, 0:256], AF.Silu), (R_ps[4],), (R["sgd"],))
            for g in range(4):
                bo = CB_BAND + (g * 3 + (2 if tt == 0 else 0)) * 128
                pe(lambda e, g=g, bo=bo, s_=s_: e.matmul(ps[5][0:64, g * 128:(g + 1) * 128], du[s_][:, g * 64:(g + 1) * 64], cb[:, bo:bo + 128],
                                                          start=(g == 0), stop=(tt == 0), skip_group_check=True), (R_du[s_], R["consts"]), (R_ps[5],))
                if tt > 0:
                    bo2 = CB_BAND + (g * 3 + 1) * 128
                    pe(lambda e, g=g, bo2=bo2, s_=s_: e.matmul(ps[5][0:64, g * 128:(g + 1) * 128], du[1 - s_][:, g * 64:(g + 1) * 64], cb[:, bo2:bo2 + 128],
                                                                start=False, stop=True, skip_group_check=True), (R_du[1 - s_], R["consts"]), (R_ps[5],))
            act(lambda e: e.copy(pooled[0:64, :], ps[5][0:64, :]), (R_ps[5],), (R["pooled"],))
            for j in range(2):
                for gg in range(2):
                    g = 2 * j + gg
                    pe(lambda e, j=j, g=g, gg=gg: e.matmul(ps[4][:, 256 + j * 128:256 + (j + 1) * 128], poolw[0:64, g * 128:(g + 1) * 128], pooled[0:64, g * 128:(g + 1) * 128],
                                                            start=False, stop=(gg == 1), skip_group_check=True), (R["pooled"], R["poolw"]), (R_ps[4],))
            for j in range(2):
                dve(lambda e, j=j, pkt=pkt: e.scalar_tensor_tensor(pkt[:, PK_G + (6 + j) * 128:PK_G + (7 + j) * 128], ps[4][:, 256 + j * 128:256 + (j + 1) * 128],
                                                                   smallp[:, SP_PS + j:SP_PS + j + 1], sgd[:, j * 128:(j + 1) * 128], ALU.mult, ALU.mult),
                    (R_ps[4], R["sgd"], R["smallp"]), (Rk["md"],))
            for b in range(11):
                bk, off = (6, b * 128) if b < 8 else (7, (b - 8) * 128)
                pe(lambda e, b=b, bk=bk, off=off: e.transpose(psb[bk][:, off:off + 128], zb[:, b * 128:(b + 1) * 128], ident), (R["zbA" if b < 7 else "zbB"], R["consts"]), (R_ps[bk],))
            MQ = r3(cb[:, CB_MQ:CB_MQ + 256], 128)
            MI = r3(cb[:, CB_MI:CB_MI + 512], 128)
            bc = lambda ap, n: ap.unsqueeze(1).broadcast_to([128, n, 128])
            for j, (bsrc, dst, key) in enumerate([(0, PK_QA, "qa"), (1, PK_QA + 256, "qa"), (3, PK_QB, "qb"), (4, PK_QB + 256, "qb")]):
                dve(lambda e, bsrc=bsrc, dst=dst, pkt=pkt: e.tensor_tensor(r3(pkt[:, dst:dst + 256], 128), bc(psb[6][:, bsrc * 128:(bsrc + 1) * 128], 2), MQ, ALU.mult),
                    (R_ps[6], R["consts"]), (Rk[key],))
            dve(lambda e, pkt=pkt: e.tensor_tensor(r3(pkt[:, PK_QI:PK_QI + 512], 128), bc(psb[6][:, 7 * 128:8 * 128], 4), MI, ALU.mult), (R_ps[6], R["consts"]), (Rk["qi"],))
            dve(lambda e, pkt=pkt: e.tensor_tensor(r3(pkt[:, PK_QI + 512:PK_QI + 1024], 128), bc(psb[7][:, 0:128], 4), MI, ALU.mult), (R_ps[7], R["consts"]), (Rk["qi"],))
            tsl = slice(tt * 128, (tt + 1) * 128)
            dve(lambda e, tsl=tsl: e.tensor_copy(kaT[:, tsl], psb[6][:, 2 * 128:3 * 128]), (R_ps[6],), ())
            dve(lambda e, tsl=tsl: e.tensor_copy(ksT[:, tsl], psb[6][:, 5 * 128:6 * 128]), (R_ps[6],), ())
            dve(lambda e, tsl=tsl: e.tensor_copy(kwT[:, tsl], psb[6][:, 6 * 128:7 * 128]), (R_ps[6],), ())
            dve(lambda e, tsl=tsl: e.tensor_copy(kiT[:, tsl], psb[7][:, 128:256]), (R_ps[7],), ())
            dve(lambda e, tsl=tsl: e.tensor_copy(kcvcT[:, tsl], psb[7][:, 256:384]), (R_ps[7],), ())
            dma(lambda e, pkt=pkt, tt=tt: e.dma_start(out=pack_d[tt, :, :], in_=pkt[:, :]), tuple(Rk.values()), (R_packd[tt],), "pst%d" % s_)
        if nt_run < NT:
            pool(lambda e: e.memset(kcvcT[:, nt_run * 128:], 0.0))
        P.barrier()
        if stop_after == "p1":
            break
        for c in range(4):
            dma(lambda e, c=c: e.dma_start(out=wst[c][:, :], in_=wcmp_d[l, :, c * 1024:(c + 1) * 1024]), (), (R_wst[c],), "wst%d" % c)
            cast(arena1[:, 16384 + c * 1024:16384 + (c + 1) * 1024], wst[c][:, :], None, (R_wst[c],), (R["wcmp"],))
        for j in range(32):
            tj = kcvc_tmp[j % 3]
            Rj = R_tmpj[j % 3]
            src = kcvcT[:, j:j + 16 * 254 + 1:16]
            pesc = smallp[:, SP_PE + j:SP_PE + j + 1]
            (pool if j % 2 == 0 else dve)(lambda e, tj=tj, src=src, pesc=pesc: e.tensor_scalar(tj[:, 0:255], src, pesc, None, ALU.add), (R["smallp"],), (Rj,))
            pe(lambda e, j=j, tj=tj: e.matmul(ps[0][:, 0:255], wcmp[0:64, j, :], tj[0:64, 0:255], start=(j == 0), stop=(j == 31)), (Rj, R["wcmp"]), (R_ps[0],))
            pe(lambda e, j=j, tj=tj: e.matmul(ps[1][:, 0:64], tj[64:128, 0:128], wcmp[64:128, j, 0:64], start=(j == 0), stop=(j == 31), skip_group_check=True), (Rj, R["wcmp"]), (R_ps[1],))
            pe(lambda e, j=j, tj=tj: e.matmul(ps[1][0:127, 64:128], tj[64:128, 128:255], wcmp[64:128, j, 0:64], start=False, stop=(j == 31), skip_group_check=True), (Rj, R["wcmp"]), (R_ps[1],))
        act(lambda e: e.copy(kc_sb[:, 0:255], ps[0][:, 0:255]), (R_ps[0],), (R["kc_sb"],))
        pe(lambda e: e.matmul(ps[2][:, 0:255], cf[:, CF_PROT:CF_PROT + 128], kc_sb[:, 0:255], start=True, stop=True), (R["kc_sb"], R["consts"]), (R_ps[2],))
        dve(lambda e: e.tensor_tensor(kc_t1[:, 0:255], kc_sb[:, 0:255], cf[:, CF_CC:CF_CC + 255], ALU.mult), (R["kc_sb"],), (R["kc_t1"],))
        dve(lambda e: e.tensor_tensor(kc_t2[:, 0:255], ps[2][:, 0:255], cf[:, CF_SC:CF_SC + 255], ALU.mult), (R_ps[2],), (R["kc_t2"],))
        dve(lambda e: e.tensor_tensor(kcmpT[:, 0:255], kc_t1[:, 0:255], kc_t2[:, 0:255], ALU.add), (R["kc_t1"], R["kc_t2"]), (R["kcmpT"],))
        act(lambda e: e.copy(vcmp[:, 0, 0:64], ps[1][:, 0:64]), (R_ps[1],), (R["vcmp"],))
        act(lambda e: e.copy(vcmp[0:127, 1, 0:64], ps[1][0:127, 64:128]), (R_ps[1],), (R["vcmp"],))
        P.barrier()
        if stop_after == "p15":
            break
        def attn_loop(kts, kT, qoff, vidx, maskfn, pks, Rpk):
            ob = 2 + obank[0] % 2
            obank[0] += 1
            acc = ps[ob]
            nk = len(kts)

            def qk(kt):
                sbk = sbank[0] % 2
                sbank[0] += 1
                ksl = slice(kt * 128, (kt + 1) * 128)
                m = maskfn(kt)
                pe(lambda e: e.matmul(ps[sbk][:, 0:256], kT[:, ksl], pks[:, qoff:qoff + 256], start=True, stop=False, skip_group_check=True), (Rpk,), (R_ps[sbk],))
                pe(lambda e: e.matmul(ps[sbk][:, 256:512], kT[:, ksl], pks[:, qoff + 256:qoff + 512], start=False, stop=(m is None), skip_group_check=True), (Rpk,), (R_ps[sbk],))
                if m is not None:
                    map_, mr = m
                    pe(lambda e: e.matmul(ps[sbk][:, 0:512], map_, I4, start=False, stop=True, skip_group_check=True), (mr, R["consts"]), (R_ps[sbk],))
                return sbk

            nxt = qk(kts[0])
            for n_, kt in enumerate(kts):
                sbk = nxt
                pi = pti[0] % 3
                pti[0] += 1
                act(lambda e: e.activation(pT[pi][:, :], ps[sbk][:, :], AF.Exp, scale=0.125), (R_ps[sbk],), (R_pT[pi],))
                if n_ + 1 < nk:
                    nxt = qk(kts[n_ + 1])
                for h in range(4):
                    pe(lambda e: e.matmul(acc[:, h * 65:(h + 1) * 65], pT[pi][:, h * 128:(h + 1) * 128], V_all[:, kt, vidx, :],
                                          start=(n_ == 0 and h == 0), stop=(n_ == nk - 1), skip_group_check=True), (R_pT[pi],), (R_ps[ob],))
            return ob

        def out_part(i):
            s_ = i % 2
            pgs = pkg[i % 3]
            Rpg = R_pkg[i % 3]
            for b in range(4):
                src = oa_bf if b < 2 else ob_bf
                pe(lambda e: e.transpose(psb[5][:, b * 128:(b + 1) * 128], src[:, (b % 2) * 128:(b % 2 + 1) * 128], ident), (R["oa"], R["ob"], R["consts"]), (R_ps[5],))
            dve(lambda e: e.tensor_tensor(mx[:, :], psb[5][:, 0:512], pgs[:, 0:512], ALU.mult), (R_ps[5], Rpg), (R["mx"],))
            for n in range(2):
                for b in range(8):
                    lh = mx[:, b * 128:(b + 1) * 128] if b < 4 else pgs[:, b * 128:(b + 1) * 128]
                    pe(lambda e: e.matmul(ps[6][:, :], lh, w_out[:, b, n * 512:(n + 1) * 512], start=(b == 0), stop=(b == 7)), (R["mx"], Rpg), (R_ps[6],))
                dve(lambda e: e.tensor_tensor(xn[:, n * 512:(n + 1) * 512], ps[6][:, :], xt[s_][:, n * 512:(n + 1) * 512], ALU.add), (R_ps[6], R_xt[s_]), (R["xn"],))
            if last and final_norm:
                rmsnorm_stats(xn[:, :], R["xn"])
                dve(lambda e: e.scalar_tensor_tensor(xt[s_][:, :], xn[:, :], RSTD, fnw[:, :], ALU.mult, ALU.mult), (R["xn"], R_sm["rstd"], R["consts"]), (R_xt[s_],))
                dma(lambda e: e.dma_start(out=out_d[i * 128:(i + 1) * 128, :], in_=xt[s_][:, :]), (R_xt[s_],), (), "ost%d" % s_)
            elif last:
                dma(lambda e: e.dma_start(out=out_d[i * 128:(i + 1) * 128, :], in_=xn[:, :]), (R["xn"],), (), "ost0")
            else:
                dma(lambda e: e.dma_start(out=xs_d[l % 2][i * 128:(i + 1) * 128, :], in_=xn[:, :]), (R["xn"],), (R_xs[l % 2][i],), "ost0")

        def idx_part(i):
            s_ = i % 2
            q0 = i * 128
            S = q0 + 128
            pks = pk[s_]
            Rpk = R_pk[s_]
            dma(lambda e: e.dma_start(out=pk[s_][:, :], in_=pack_d[i, :, 0:2048]), (R_packd[i],), (R_pk[s_],), "pk%d" % s_)
            dma(lambda e: e.dma_start(out=pkg[i % 3][:, :], in_=pack_d[i, :, 2048:3072]), (R_packd[i],), (R_pkg[i % 3],), "pkg%d" % (i % 3))
            if i < 2:
                return
            dve(lambda e: e.tensor_tensor(diagW[:, :, :], ident.unsqueeze(1).broadcast_to([128, 8, 128]),
                                          wi_all[:, i, :].unsqueeze(2).broadcast_to([128, 8, 128]), ALU.mult), (R["consts"],), (R["diagW"],))
            nch = (S + 511) // 512
            for c in range(nch):
                c0 = c * 512
                w = min(512, S - c0)
                pend = []
                for h in range(8):
                    sbk = (0, 1, 7, 6)[ibank[0] % 4]
                    ibank[0] += 1
                    ri = rti[0] % 4
                    rti[0] += 1
                    pe(lambda e: e.matmul(ps[sbk][:, 0:w], pks[:, PK_QI + h * 128:PK_QI + (h + 1) * 128], kiT[:, c0:c0 + w], start=True, stop=True),
                       (Rpk,), (R_ps[sbk],))
                    if h % 3 == 2:
                        dve(lambda e: e.tensor_scalar(Rt[ri][:, 0:w], ps[sbk][:, 0:w], 0.0, None, ALU.max), (R_ps[sbk],), (R_Rt[ri],))
                    else:
                        act(lambda e: e.activation(Rt[ri][:, 0:w], ps[sbk][:, 0:w], AF.Relu), (R_ps[sbk],), (R_Rt[ri],))
                    pend.append((h, ri))
                    if len(pend) > 3:
                        ph, pri = pend.pop(0)
                        pe(lambda e: e.matmul(ps[4][:, 0:w], diagW[:, ph, :], Rt[pri][:, 0:w], start=(ph == 0), stop=False), (R_Rt[pri], R["diagW"]), (R_ps[4],))
                for ph, pri in pend:
                    pe(lambda e: e.matmul(ps[4][:, 0:w], diagW[:, ph, :], Rt[pri][:, 0:w], start=(ph == 0), stop=(ph == 7)), (R_Rt[pri], R["diagW"]), (R_ps[4],))
                dve(lambda e: e.tensor_copy(score[:, c0:c0 + w], ps[4][:, 0:w]), (R_ps[4],), (R["score"],))

        def bis_part(i):
            s_ = i % 2
            q0 = i * 128
            S = q0 + 128
            maskA = maskA2[s_]
            RmA = R_mA[s_]
            if i < 2:
                if q0 > 0:
                    pool(lambda e: e.memset(maskA[:, 0:q0], 0.0), (), (RmA,))
                pool(lambda e: e.tensor_copy(maskA[:, q0:q0 + 128], causb), (R["consts"],), (RmA,))
                return
            dve(lambda e: e.tensor_tensor(score[:, q0:q0 + 128], score[:, q0:q0 + 128], causb, ALU.add), (R["score"], R["consts"]), (R["score"],))
            dve(lambda e: e.tensor_reduce(HI, score[:, 0:S], AX.X, ALU.max), (R["score"],), (R_sm["hi"],))
            dve(lambda e: e.tensor_reduce(LO, score[:, 0:256], AX.X, ALU.min), (R["score"],), (R_sm["lo"],))
            dve(lambda e: e.tensor_tensor(W0, HI, LO, ALU.subtract), (R_sm["hi"], R_sm["lo"]), (R_sm["w0"],))
            dve(lambda e: e.tensor_scalar(WK, cf[:, CF_POW:CF_POW + 16], W0, None, ALU.mult), (R_sm["w0"], R["consts"]), (R_sm["wk"],))
            dve(lambda e: e.tensor_tensor(TB[0], LO, WK[:, 0:1], ALU.add), (R_sm["lo"], R_sm["wk"]), (R_sm["t0"],))
            for k_ in range(NBIS):
                ta, tb_ = TB[k_ % 2], TB[(k_ + 1) % 2]
                Ra, Rb = R_sm["t%d" % (k_ % 2)], R_sm["t%d" % ((k_ + 1) % 2)]
                dve(lambda e: e.tensor_scalar(junk[:, 0:S], score[:, 0:S], ta, 0.0, ALU.is_ge, ALU.add, accum_out=CNT), (R["score"], Ra), (R["junk"], R_sm["cnt"]))
                dve(lambda e: e.tensor_scalar(UU, CNT, TOPK - 0.5, WK[:, k_:k_ + 1], ALU.is_ge, ALU.mult), (R_sm["cnt"], R_sm["wk"]), (R_sm["uu"],))
                dve(lambda e: e.scalar_tensor_tensor(tb_, UU, WK[:, k_ + 1:k_ + 2], ta, ALU.subtract, ALU.add), (R_sm["uu"], R_sm["wk"], Ra), (Rb,))
            tf = TB[NBIS % 2]
            Rf = R_sm["t%d" % (NBIS % 2)]
            dve(lambda e: e.tensor_scalar(maskA[:, 0:S], score[:, 0:S], tf, NEG, ALU.is_lt, ALU.mult), (R["score"], Rf), (RmA,))

        idx_part(0)
        bis_part(0)
        for i in range(nt_run):
            s_ = i % 2
            q0 = i * 128
            S = q0 + 128
            pks = pk[s_]
            Rpk = R_pk[s_]
            maskA, maskB = maskA2[s_], maskB2[s_]
            R["maskA"], R["maskB"] = R_mA[s_], R_mB[s_]
            dma(lambda e, s_=s_, i=i: e.dma_start(out=xt[s_][:, :], in_=xsrc[i * 128:(i + 1) * 128, :]),
                () if R_xsrc is None else (R_xsrc[i],), (R_xt[s_],), "xt%d" % s_)
            if i + 1 < nt_run:
                idx_part(i + 1)
            if i > 0:
                out_part(i - 1)
            if i > 0:
                pool(lambda e, i=i: e.memset(maskC[:, 8 * (i - 1):8 * (i - 1) + 8], 0.0), (), (R["maskC"],))
            pool(lambda e, i=i: e.tensor_copy(maskC[:, 8 * i:8 * i + 8], cb[:, CB_P8:CB_P8 + 8]), (R["consts"],), (R["maskC"],))
            ntc = 1 if 8 * i + 7 <= 128 else 2
            obu = 2 + obank[0] % 2
            obank[0] += 1
            for nt in range(ntc):
                sbk = sbank[0] % 2
                sbank[0] += 1
                nsl = slice(nt * 128, (nt + 1) * 128)
                pe(lambda e, sbk=sbk, nsl=nsl: e.matmul(ps[sbk][:, 0:256], kcmpT[:, nsl], pks[:, PK_QB:PK_QB + 256], start=True, stop=False, skip_group_check=True), (Rpk, R["kcmpT"]), (R_ps[sbk],))
                pe(lambda e, sbk=sbk, nsl=nsl: e.matmul(ps[sbk][:, 256:512], kcmpT[:, nsl], pks[:, PK_QB + 256:PK_QB + 512], start=False, stop=False, skip_group_check=True), (Rpk,), (R_ps[sbk],))
                pe(lambda e, sbk=sbk, nt=nt: e.matmul(ps[sbk][:, 0:512], maskC[:, 1 + nt * 128:1 + (nt + 1) * 128], I4, start=False, stop=True, skip_group_check=True), (R["maskC"], R["consts"]), (R_ps[sbk],))
                pi = pti[0] % 3
                pti[0] += 1
                act(lambda e, sbk=sbk, pi=pi: e.activation(pT[pi][:, :], ps[sbk][:, :], AF.Exp, scale=0.125), (R_ps[sbk],), (R_pT[pi],))
                for h in range(4):
                    pe(lambda e, h=h, pi=pi, nt=nt: e.matmul(ps[obu][:, h * 128:(h + 1) * 128], pT[pi][:, h * 128:(h + 1) * 128], vcmp[:, nt, :],
                                                              start=(nt == 0 and h == 0), stop=(nt == ntc - 1), skip_group_check=True), (R_pT[pi], R["vcmp"]), (R_ps[obu],))
            Uv = r3(ps[obu][:, :], 128)
            dve(lambda e, Uv=Uv: e.tensor_scalar(DENC.unsqueeze(2), Uv[:, :, 64:65], 1e-30, None, ALU.max), (R_ps[obu],), (R_sm["denc"],))
            dve(lambda e: e.reciprocal(RDC, DENC), (R_sm["denc"],), (R_sm["rdc"],))
            dve(lambda e, Uv=Uv: e.tensor_scalar(imp[:, 1:64], Uv[:, 0, 65:128], RDC[:, 0:1], None, ALU.mult), (R_ps[obu], R_sm["rdc"]), (R["imp"],))
            for h in range(1, 4):
                dve(lambda e, Uv=Uv, h=h: e.scalar_tensor_tensor(imp[:, 1:64], Uv[:, h, 65:128], RDC[:, h:h + 1], imp[:, 1:64], ALU.mult, ALU.add), (R_ps[obu], R_sm["rdc"], R["imp"]), (R["imp"],))
            ko = 62 - 2 * i
            dve(lambda e, ko=ko: e.tensor_tensor(imp2[:, :], imp[:, :], cf[:, CF_KEEP + ko:CF_KEEP + ko + 64], ALU.mult), (R["imp"], R["consts"]), (R["imp2"],))
            dve(lambda e, ko=ko: e.tensor_tensor(imp2[:, :], imp2[:, :], cf[:, CF_ADD + ko:CF_ADD + ko + 64], ALU.add), (R["imp2"],), (R["imp2"],))
            dve(lambda e: e.max(M8A, imp2[:, :]), (R["imp2"],), (R_sm["m8a"],))
            dve(lambda e: e.match_replace(imp3[:, :], M8A, imp2[:, :], -3.0e38), (R["imp2"], R_sm["m8a"]), (R["imp3"],))
            dve(lambda e: e.max(M8B, imp3[:, :]), (R["imp3"],), (R_sm["m8b"],))
            nb = 2 * (i + 1)
            dve(lambda e, nb=nb, S=S: e.tensor_scalar(r3(maskB[:, 0:S], 64), imp2[:, 0:nb].unsqueeze(2).broadcast_to([128, nb, 64]), M8B[:, 7:8], NEG, ALU.is_lt, ALU.mult),
                (R["imp2"], R_sm["m8b"]), (R["maskB"],))
            dve(lambda e, q0=q0: e.tensor_tensor(maskB[:, q0:q0 + 128], maskB[:, q0:q0 + 128], causb, ALU.add), (R["maskB"], R["consts"]), (R["maskB"],))
            gv = r3(g_all[:, i, :], 3)
            dve(lambda e, gv=gv: e.tensor_tensor(FC.unsqueeze(2), gv[:, :, 0:1], RDC.unsqueeze(2), ALU.mult), (R_sm["rdc"],), (R_sm["fc"],))
            dve(lambda e, Uv=Uv: e.tensor_tensor(r3(ob32[:, :], 64), Uv[:, :, 0:64], FC.unsqueeze(2).broadcast_to([128, 4, 64]), ALU.mult), (R_ps[obu], R_sm["fc"]), (R["ob32"],))
            oba = attn_loop(list(range(i + 1)), kaT, PK_QA, 0, lambda kt: (maskA[:, kt * 128:(kt + 1) * 128], R["maskA"]), pks, Rpk)
            acc = r3(ps[oba][:, 0:260], 65)
            dve(lambda e, acc=acc: e.reciprocal(RDA.unsqueeze(2), acc[:, :, 64:65]), (R_ps[oba],), (R_sm["rda"],))
            dve(lambda e, acc=acc: e.tensor_tensor(r3(oa_bf[:, :], 64), acc[:, :, 0:64], RDA.unsqueeze(2).broadcast_to([128, 4, 64]), ALU.mult), (R_ps[oba], R_sm["rda"]), (R["oa"],))
            if i + 1 < nt_run:
                bis_part(i + 1)
            obs = attn_loop(list(range(i + 1)), ksT, PK_QB, 1, lambda kt: (maskB[:, kt * 128:(kt + 1) * 128], R["maskB"]), pks, Rpk)
            acc = r3(ps[obs][:, 0:260], 65)
            dve(lambda e, acc=acc: e.reciprocal(RDS.unsqueeze(2), acc[:, :, 64:65]), (R_ps[obs],), (R_sm["rds"],))
            dve(lambda e, gv=gv: e.tensor_tensor(FS.unsqueeze(2), gv[:, :, 1:2], RDS.unsqueeze(2), ALU.mult), (R_sm["rds"],), (R_sm["fs"],))
            dve(lambda e, acc=acc: e.tensor_tensor(r3(tmp32[:, :], 64), acc[:, :, 0:64], FS.unsqueeze(2).broadcast_to([128, 4, 64]), ALU.mult), (R_ps[obs], R_sm["fs"]), (R["tmp32"],))
            dve(lambda e: e.tensor_tensor(ob32[:, :], ob32[:, :], tmp32[:, :], ALU.add), (R["ob32"], R["tmp32"]), (R["ob32"],))
            def wmask(kt, i=i):
                d_ = i - kt
                if d_ == 0:
                    return (causb, R["consts"])
                if d_ == 4:
                    return (win4b, R["consts"])
                return None
            obw = attn_loop(list(range(max(0, i - 4), i + 1)), kwT, PK_QB, 2, wmask, pks, Rpk)
            acc = r3(ps[obw][:, 0:260], 65)
            dve(lambda e, acc=acc: e.reciprocal(RDW.unsqueeze(2), acc[:, :, 64:65]), (R_ps[obw],), (R_sm["rdw"],))
            dve(lambda e, gv=gv: e.tensor_tensor(FW.unsqueeze(2), gv[:, :, 2:3], RDW.unsqueeze(2), ALU.mult), (R_sm["rdw"],), (R_sm["fw"],))
            dve(lambda e, acc=acc: e.tensor_tensor(r3(tmp32[:, :], 64), acc[:, :, 0:64], FW.unsqueeze(2).broadcast_to([128, 4, 64]), ALU.mult), (R_ps[obw], R_sm["fw"]), (R["tmp32"],))
            dve(lambda e: e.tensor_tensor(ob_bf[:, :], ob32[:, :], tmp32[:, :], ALU.add), (R["ob32"], R["tmp32"]), (R["ob"],))
        out_part(nt_run - 1)
        P.barrier()
    P.build(nc, es)
    es.close()
    return nc


_CACHE = {}


def _host_prep(norm_w, w_in, w_out, conv_w, pe_cmp, w_cmp_k, w_cmp_v, pool_w, pool_scale, final_norm_w):
    ci = _colidx()
    w_in_p = np.ascontiguousarray(np.asarray(w_in, np.float32)[:, :, ci])
    smallp = np.zeros((DEPTH, 128, SPW), np.float32)
    wcmp = np.zeros((DEPTH, 128, 32, 128), np.float32)
    for l in range(DEPTH):
        smallp[l, :, SP_NW:SP_NW + 8] = np.asarray(norm_w[l]).reshape(8, 128).T
        smallp[l, :, SP_CONV:SP_CONV + 6] = np.asarray(conv_w[l]).T.reshape(2, 128, 3).transpose(1, 0, 2).reshape(128, 6)
        smallp[l, :, SP_PE:SP_PE + 32] = np.tile(np.asarray(pe_cmp[l]).T, (2, 1))
        smallp[l, :, SP_PS:SP_PS + 2] = np.asarray(pool_scale[l]).reshape(2, 128).T
        for g in range(4):
            o = SP_POOLW + g * 128 + (g % 2) * 64
            smallp[l, 0:64, o:o + 64] = np.asarray(pool_w[l, g])
        wk = np.asarray(w_cmp_k[l]).reshape(32, 64, 64).transpose(1, 0, 2)
        wv = np.asarray(w_cmp_v[l]).reshape(32, 64, 64).transpose(1, 0, 2)
        wcmp[l, 0:64, :, 0:64] = wk
        wcmp[l, 0:64, :, 64:128] = wk
        wcmp[l, 64:128, :, 0:64] = wv
    return dict(w_in=w_in_p, w_out=np.ascontiguousarray(np.asarray(w_out, np.float32)), wcmp=wcmp.reshape(DEPTH, 128, 4096),
                smallp=smallp, fnw=np.asarray(final_norm_w, np.float32).reshape(1, D))


def run(inputs, n_layers=DEPTH, final_norm=True, cores=8, dbg_pack=False, trace=False, nt_run=NT, stop_after=None, p1_cut=None):
    key = (n_layers, final_norm, dbg_pack, nt_run, stop_after, p1_cut)
    if key not in _CACHE:
        _CACHE[key] = build_program(n_layers, final_norm, dbg_pack, nt_run, stop_after, p1_cut)
    nc = _CACHE[key]
    shared = _host_prep(*[inputs[k] for k in ["norm_w", "w_in", "w_out", "conv_w", "pe_cmp", "w_cmp_k", "w_cmp_v", "pool_w", "pool_scale", "final_norm_w"]])
    cfc, cbc = _consts()
    shared["cst_f"] = cfc
    shared["cst_b"] = cbc
    x = np.asarray(inputs["x"], np.float32)
    in_maps = []
    for c in range(cores):
        m = dict(shared)
        m["x"] = np.ascontiguousarray(x[c])
        in_maps.append(m)
    kw = {"trace": True} if trace else {}
    res = run_bass_kernel_spmd(nc, in_maps, core_ids=list(range(cores)), **kw)
    return res


def kernel(x, norm_w, w_in, w_out, conv_w, pe_cmp, w_cmp_k, w_cmp_v, pool_w, pool_scale, final_norm_w):
    inputs = dict(x=x, norm_w=norm_w, w_in=w_in, w_out=w_out, conv_w=conv_w, pe_cmp=pe_cmp, w_cmp_k=w_cmp_k,
                  w_cmp_v=w_cmp_v, pool_w=pool_w, pool_scale=pool_scale, final_norm_w=final_norm_w)
    res = run(inputs)
    return np.stack([np.asarray(r["out"], np.float32) for r in res.results], axis=0)
```
